# Optimizing a Trainium2 kernel written in Bass

```python
import math
import jax, jax.numpy as jnp
from jax import lax
import numpy as np

D_MODEL = 1024
BATCH = 4
SEQ = 8192
DEPTH = 4

HEAD_DIM = 64
A_HEADS = 4
B_HEADS = 4
C_HEADS = 4
A_WIDTH = A_HEADS * HEAD_DIM
HGRN_EXPAND = 64
B_KEY_WIDTH = B_HEADS * HGRN_EXPAND
B_WIDTH = B_HEADS * HEAD_DIM
C_QK_WIDTH = C_HEADS * 2 * HEAD_DIM
C_VDIM = 2 * HEAD_DIM
C_WIDTH = C_HEADS * C_VDIM
D_MIX = A_WIDTH + B_WIDTH + C_WIDTH
SPLIT_SIZES = (A_WIDTH, A_WIDTH, A_WIDTH,
               B_KEY_WIDTH, B_KEY_WIDTH, B_KEY_WIDTH,
               B_WIDTH, B_WIDTH,
               C_QK_WIDTH, C_QK_WIDTH, C_WIDTH)
IN_COLS = sum(SPLIT_SIZES)
ROPE_THETA = 500000.0
ROPE_DIM = HEAD_DIM // 4
DILATED_PAIRS = ((128, 1), (512, 4), (2048, 16))
SWA_BLOCK = 64
HGRN_CHUNK = 64
Q_BLOCK = 128
D_FF = 2816
CONV_WIDTH = 3
EPS = 1e-6
NEG_INF = -1e30

kernel_name = "hymba_style_dilated_hgrn2_diffattn_encoder"


def rms_norm(x, g):
    xf = x.astype(jnp.float32)
    y = xf * lax.rsqrt(jnp.mean(xf * xf, axis=-1, keepdims=True) + EPS)
    return (y * g.astype(jnp.float32)).astype(x.dtype)


def split_cols(h):
    outs, start = [], 0
    for size in SPLIT_SIZES:
        outs.append(h[..., start:start + size])
        start += size
    return outs


def rope_tables(seq):
    pos = jnp.arange(seq, dtype=jnp.float32)
    inv = ROPE_THETA ** (-jnp.arange(0, ROPE_DIM, 2, dtype=jnp.float32) / ROPE_DIM)
    ang = pos[:, None] * inv[None, :]
    return jnp.cos(ang), jnp.sin(ang)


def partial_rope(x, cos, sin):
    half = ROPE_DIM // 2
    x1, x2, rest = x[..., :half], x[..., half:ROPE_DIM], x[..., ROPE_DIM:]
    return jnp.concatenate([x1 * cos - x2 * sin, x2 * cos + x1 * sin, rest], axis=-1)


def heads(t, n):
    b, s, w = t.shape
    return t.reshape(b, s, n, w // n).transpose(0, 2, 1, 3)


def dilated_branch(q, k, v, window, dilation):
    b, h, s, hd = q.shape
    half = window // (2 * dilation)
    L = s // dilation
    nb = -(-L // SWA_BLOCK)
    Lp = nb * SWA_BLOCK

    def to_sub(t):
        return t.reshape(b, h, L, dilation, hd).transpose(0, 1, 3, 2, 4)

    qs = jnp.pad(to_sub(q), ((0, 0), (0, 0), (0, 0), (0, Lp - L), (0, 0)))
    pad_k = ((0, 0), (0, 0), (0, 0), (SWA_BLOCK, Lp - L + SWA_BLOCK), (0, 0))
    ks = jnp.pad(to_sub(k), pad_k)
    vs = jnp.pad(to_sub(v), pad_k)

    def windows(t):
        t = t.reshape(b, h, dilation, nb + 2, SWA_BLOCK, hd)
        return jnp.concatenate([t[:, :, :, 0:nb], t[:, :, :, 1:nb + 1], t[:, :, :, 2:nb + 2]], axis=-2)

    kw, vw = windows(ks), windows(vs)
    qb = qs.reshape(b, h, dilation, nb, SWA_BLOCK, hd)
    sc = jnp.einsum('bhrnqd,bhrnkd->bhrnqk', qb, kw) * (1.0 / math.sqrt(hd))
    blk = jnp.arange(nb)[:, None, None]
    qpos = blk * SWA_BLOCK + jnp.arange(SWA_BLOCK)[None, :, None]
    kpos = (blk - 1) * SWA_BLOCK + jnp.arange(3 * SWA_BLOCK)[None, None, :]
    mask = (jnp.abs(kpos - qpos) <= half) & (kpos >= 0) & (kpos < L)
    sc = jnp.where(mask, sc, NEG_INF)
    m = jnp.max(sc, axis=-1, keepdims=True)
    p = jnp.exp(sc - m)
    den = jnp.sum(p, axis=-1)
    o = jnp.einsum('bhrnqk,bhrnkd->bhrnqd', p, vw) / den[..., None]
    lse = m[..., 0] + jnp.log(den)
    o = o.reshape(b, h, dilation, Lp, hd)[:, :, :, :L].transpose(0, 1, 3, 2, 4).reshape(b, h, s, hd)
    lse = lse.reshape(b, h, dilation, Lp)[..., :L].transpose(0, 1, 3, 2).reshape(b, h, s)
    return o, lse


def dilated_mixer(q, k, v):
    outs, lses = [], []
    for window, dilation in DILATED_PAIRS:
        o, lse = dilated_branch(q, k, v, window, dilation)
        outs.append(o)
        lses.append(lse)
    w = jax.nn.softmax(jnp.stack(lses, axis=0), axis=0)
    return jnp.sum(w[..., None] * jnp.stack(outs, axis=0), axis=0)


def hgrn_scan(q, k, v, logf):
    b, h, s, dk = q.shape
    dv = v.shape[-1]
    nc = s // HGRN_CHUNK

    def chunks(t):
        return t.reshape(b, h, nc, HGRN_CHUNK, t.shape[-1]).transpose(2, 0, 1, 3, 4)

    tri = jnp.tril(jnp.ones((HGRN_CHUNK, HGRN_CHUNK), dtype=bool))[:, :, None]

    def step(state, inp):
        qc, kc, vc, gc = inp
        a = jnp.cumsum(gc, axis=-2)
        inter = jnp.einsum('bhtd,bhdv->bhtv', qc * jnp.exp(a), state)
        diff = a[:, :, :, None, :] - a[:, :, None, :, :]
        decay = jnp.exp(jnp.where(tri, diff, -jnp.inf))
        scores = jnp.einsum('bhtsd,bhsd->bhts', qc[:, :, :, None, :] * decay, kc)
        intra = jnp.einsum('bhts,bhsv->bhtv', scores, vc)
        a_last = a[:, :, -1:, :]
        new_state = (jnp.exp(a_last[:, :, 0, :])[..., None] * state
                     + jnp.einsum('bhsd,bhsv->bhdv', kc * jnp.exp(a_last - a), vc))
        return new_state, inter + intra

    init = jnp.zeros((b, h, dk, dv), jnp.float32)
    _, out = lax.scan(step, init, (chunks(q), chunks(k), chunks(v), chunks(logf)))
    return out.transpose(1, 2, 0, 3, 4).reshape(b, h, s, dv)


def hgrn_forget(z, lb):
    z = z.astype(jnp.float32)
    f = lb + (1.0 - lb) * jax.nn.sigmoid(z)
    return jnp.log(f), (1.0 - lb) * jax.nn.sigmoid(-z)


def diff_attention(q, k, v, lam):
    b, h, _, s, hd = q.shape
    nq = s // Q_BLOCK
    qb = q.reshape(b, h, 2, nq, Q_BLOCK, hd).transpose(3, 0, 1, 2, 4, 5)
    scale = 1.0 / math.sqrt(hd)

    def block(qblk):
        sc = jnp.einsum('bhcqd,bhckd->bhcqk', qblk, k) * scale
        a = jax.nn.softmax(sc.astype(jnp.float32), axis=-1)
        w = a[:, :, 0] - lam * a[:, :, 1]
        return jnp.einsum('bhqk,bhkv->bhqv', w, v)

    o = lax.map(block, qb)
    return o.transpose(1, 2, 0, 3, 4).reshape(b, h, s, v.shape[-1])


def mixer_layer(xn, w_in_l, w_out_l, lb_f, lb_b, hgrn_g, lam_p, diff_g, layer, cos, sin):
    b, s, _ = xn.shape
    (qa, ka, va, qb_, fzf, fzb, ib, gb, qc, kc, vc) = split_cols(xn @ w_in_l)

    qa = partial_rope(heads(qa, A_HEADS).astype(jnp.float32), cos, sin)
    ka = partial_rope(heads(ka, A_HEADS).astype(jnp.float32), cos, sin)
    oa = dilated_mixer(qa, ka, heads(va, A_HEADS).astype(jnp.float32))
    oa = oa.transpose(0, 2, 1, 3).reshape(b, s, A_WIDTH).astype(xn.dtype)

    logf_f, k_f = hgrn_forget(fzf, lb_f)
    logf_b, k_b = hgrn_forget(fzb, lb_b)
    qh = heads(qb_, B_HEADS).astype(jnp.float32)
    vh = heads(ib, B_HEADS).astype(jnp.float32)
    o_fwd = hgrn_scan(qh, heads(k_f, B_HEADS), vh, heads(logf_f, B_HEADS))
    flip = lambda t: jnp.flip(t, axis=2)
    o_bwd = flip(hgrn_scan(flip(qh), flip(heads(k_b, B_HEADS)), flip(vh), flip(heads(logf_b, B_HEADS))))
    ob = (o_fwd + o_bwd).transpose(0, 2, 1, 3)
    ob = rms_norm(ob, hgrn_g) * jax.nn.silu(gb.astype(jnp.float32)).reshape(b, s, B_HEADS, HEAD_DIM)
    ob = ob.reshape(b, s, B_WIDTH).astype(xn.dtype)

    lam_init = 0.8 - 0.6 * math.exp(-0.3 * layer)
    lp = lam_p.astype(jnp.float32)
    lam = jnp.exp(jnp.sum(lp[0] * lp[1])) - jnp.exp(jnp.sum(lp[2] * lp[3])) + lam_init
    qc = qc.reshape(b, s, C_HEADS, 2, HEAD_DIM).transpose(0, 2, 3, 1, 4).astype(jnp.float32)
    kc = kc.reshape(b, s, C_HEADS, 2, HEAD_DIM).transpose(0, 2, 3, 1, 4).astype(jnp.float32)
    qc = partial_rope(qc, cos, sin)
    kc = partial_rope(kc, cos, sin)
    oc = diff_attention(qc, kc, heads(vc, C_HEADS).astype(jnp.float32), lam)
    oc = rms_norm(oc.transpose(0, 2, 1, 3), diff_g) * (1.0 - lam_init)
    oc = oc.reshape(b, s, C_WIDTH).astype(xn.dtype)

    return jnp.concatenate([oa, ob, oc], axis=-1) @ w_out_l


def conv_ffn(xn, w_up_l, cw, cb, w_down_l):
    u = xn @ w_up_l
    up = jnp.pad(u, ((0, 0), (1, 1), (0, 0)))
    u = up[:, :-2] * cw[0] + up[:, 1:-1] * cw[1] + up[:, 2:] * cw[2] + cb
    gate, val = jnp.split(u, 2, axis=-1)
    return (jax.nn.silu(gate) * val) @ w_down_l


def setup_inputs(seed: int = 0) -> dict:
    key = jax.random.key(seed)
    ks = jax.random.split(key, 16)
    f32 = jnp.float32
    nrm = lambda k, shape, scale: jax.random.normal(k, shape, f32) * scale
    gain = lambda k, shape: 1.0 + 0.05 * jax.random.normal(k, shape, f32)
    return {
        "x": jax.random.normal(ks[0], (BATCH, SEQ, D_MODEL), f32),
        "w_in": nrm(ks[1], (DEPTH, D_MODEL, IN_COLS), D_MODEL ** -0.5),
        "w_out": nrm(ks[2], (DEPTH, D_MIX, D_MODEL), D_MIX ** -0.5),
        "lb_logits": nrm(ks[3], (2, DEPTH, B_KEY_WIDTH), 0.1),
        "hgrn_norm": gain(ks[4], (DEPTH, HEAD_DIM)),
        "diff_lambda": nrm(ks[5], (DEPTH, 4, HEAD_DIM), 0.1),
        "diff_norm": gain(ks[6], (DEPTH, C_VDIM)),
        "w_up": nrm(ks[7], (DEPTH, D_MODEL, 2 * D_FF), D_MODEL ** -0.5),
        "conv_w": nrm(ks[8], (DEPTH, CONV_WIDTH, 2 * D_FF), CONV_WIDTH ** -0.5),
        "conv_b": nrm(ks[9], (DEPTH, 2 * D_FF), 0.02),
        "w_down": nrm(ks[10], (DEPTH, D_FF, D_MODEL), D_FF ** -0.5),
        "norm_pre_mix": gain(ks[11], (DEPTH, D_MODEL)),
        "norm_post_mix": gain(ks[12], (DEPTH, D_MODEL)),
        "norm_pre_ffn": gain(ks[13], (DEPTH, D_MODEL)),
        "norm_post_ffn": gain(ks[14], (DEPTH, D_MODEL)),
    }


def reference(x, w_in, w_out, lb_logits, hgrn_norm, diff_lambda, diff_norm, w_up, conv_w, conv_b,
              w_down, norm_pre_mix, norm_post_mix, norm_pre_ffn, norm_post_ffn):
    cos, sin = rope_tables(x.shape[1])
    p = jax.nn.softmax(lb_logits.astype(jnp.float32), axis=1)
    lbs = jnp.cumsum(p, axis=1) - p[:, :1]
    for l in range(DEPTH):
        xn = rms_norm(x, norm_pre_mix[l])
        mix = mixer_layer(xn, w_in[l], w_out[l], lbs[0, l], lbs[1, l], hgrn_norm[l],
                          diff_lambda[l], diff_norm[l], l, cos, sin)
        x = x + rms_norm(mix, norm_post_mix[l])
        xn = rms_norm(x, norm_pre_ffn[l])
        ff = conv_ffn(xn, w_up[l], conv_w[l], conv_b[l], w_down[l])
        x = x + rms_norm(ff, norm_post_ffn[l])
    return x
```

```python
import math
import numpy as np
import concourse.bass as bass
import concourse.mybir as mybir
from concourse.bass_utils import run_bass_kernel_spmd
from contextlib import ExitStack

F32 = mybir.dt.float32
BF16 = mybir.dt.bfloat16
AF = mybir.ActivationFunctionType
ALU = mybir.AluOpType
AX = mybir.AxisListType

S = 8192
D = 1024
NT = S // 128
NB = S // 512
DFF = 2816
NPP = 468
NCST = 1280
EPS = 1e-6


def _is_psum(name):
    return name == "bT" or (len(name) == 2 and name[0] in "bsodmfh" and name[1].isdigit())


class Buf:
    __slots__ = ("name", "w", "rs")

    def __init__(self, name):
        self.name = name
        self.w = None
        self.rs = []


class Prog:
    NDMA = 8

    def __init__(self, nc, es):
        self.nc = nc
        self.sem = {e: es.enter_context(nc.semaphore("c_" + e)) for e in ("pe", "act", "dve", "pool")}
        self.cnt = {e: 0 for e in self.sem}
        self.dq = {}
        for q in ("sp", "act", "pool"):
            self.dq[q] = dict(sems=[es.enter_context(nc.semaphore("d_%s%d" % (q, i))) for i in range(self.NDMA)], n=0)
        self.stream = {e: [] for e in ("pe", "act", "dve", "pool", "sp")}
        self.waited = {}
        self.bufs = {}
        import os
        self.maxops = int(os.environ.get("KMAXOPS", "0"))
        self.nops = 0

    def buf(self, name):
        b = self.bufs.get(name)
        if b is None:
            b = self.bufs[name] = Buf(name)
        return b

    def _need(self, e, toks):
        out = {}
        for t in toks:
            if t is None:
                continue
            sem, val, _ = t
            if self.waited.get((e, id(sem)), 0) >= val:
                continue
            if id(sem) not in out or out[id(sem)][1] < val:
                out[id(sem)] = (sem, val)
        for sid, (sem, val) in out.items():
            self.waited[(e, sid)] = val
        return list(out.values())

    def _deps(self, reads, writes):
        toks = []
        for b in reads:
            toks.append(b.w)
        for b in writes:
            toks.append(b.w)
            toks.extend(b.rs)
        return toks

    def _commit(self, tok, reads, writes):
        for b in reads:
            b.rs.append(tok)
            if len(b.rs) > 64:
                b.rs = b.rs[-64:] if False else b.rs
        for b in writes:
            b.w = tok
            b.rs = []

    def op(self, e, fn, reads=(), writes=()):
        self.nops += 1
        if self.maxops:
            import sys
            f = sys._getframe(1)
            f2 = f.f_back
            print("OP", self.nops, e, f.f_code.co_name, f.f_lineno, f2.f_lineno, list(reads), list(writes))
        if self.maxops and self.nops > self.maxops:
            return None
        writes = list(writes) + [b for b in reads if _is_psum(b)]
        reads = [self.buf(b) for b in reads if not _is_psum(b)]
        writes = [self.buf(b) for b in writes]
        toks = self._deps(reads, writes)
        if e == "pe":
            toks = [t for t in toks if t is not None and t[2] != "pe"]
        waits = self._need(e, toks)
        self.cnt[e] += 1
        tok = (self.sem[e], self.cnt[e], e)
        self.stream[e].append((waits, fn, (self.sem[e], 1)))
        self._commit(tok, reads, writes)
        return tok

    def dma(self, out, in_, reads=(), writes=(), q="sp"):
        self.nops += 1
        if self.maxops:
            import sys
            f = sys._getframe(1)
            print("OP", self.nops, "dma", f.f_lineno, list(reads), list(writes))
        if self.maxops and self.nops > self.maxops:
            return None
        reads = [self.buf(b) for b in reads]
        writes = [self.buf(b) for b in writes]
        d = self.dq[q]
        i = d["n"]
        d["n"] += 1
        sem = d["sems"][i % self.NDMA]
        rnd = i // self.NDMA
        toks = self._deps(reads, writes)
        if rnd > 0:
            toks.append((sem, 16 * rnd, q))
        waits = self._need(q, toks)
        tok = (sem, 16 * (rnd + 1), q)
        self.stream[q].append((waits, (lambda eng, out=out, in_=in_: eng.dma_start(out=out, in_=in_)), (sem, 16)))
        self._commit(tok, reads, writes)
        return tok

    def barrier(self):
        import os
        if os.environ.get("KVERBOSE"):
            print("barrier nops", self.nops, dict(self.cnt), {q: d["n"] for q, d in self.dq.items()}, flush=True)
        toks = [(self.sem[e], self.cnt[e], e) for e in self.sem if self.cnt[e] > 0]
        for q, d in self.dq.items():
            n = d["n"]
            for j in range(min(n, self.NDMA)):
                last = ((n - 1 - j) // self.NDMA) * self.NDMA + j
                cntj = (n - 1 - j) // self.NDMA + 1
                toks.append((d["sems"][j], 16 * cntj, q))
        for e in ("pe", "act", "dve", "pool", "sp"):
            waits = self._need(e, toks)
            if waits:
                self.stream[e].append((waits, None, None))
        for b in self.bufs.values():
            b.w = None
            b.rs = []

    def emit(self):
        nc = self.nc
        self.barrier()
        csem = {id(v): k for k, v in self.sem.items()}
        sig = {e: set() for e in self.sem}
        for e in self.stream:
            for waits, fn, inc in self.stream[e]:
                for sem, val in waits:
                    if id(sem) in csem:
                        sig[csem[id(sem)]].add(val)
        rank = {e: {v: i + 1 for i, v in enumerate(sorted(sig[e]))} for e in sig}
        with nc.Block() as block:
            def run(e):
                def body(eng):
                    idx = 0
                    for waits, fn, inc in self.stream[e]:
                        for sem, val in waits:
                            if id(sem) in csem:
                                val = rank[csem[id(sem)]][val]
                            eng.wait_ge(sem, val)
                        if fn is not None:
                            ins = fn(eng)
                            if inc[1] == 16:
                                ins.then_inc(inc[0], 16)
                            else:
                                idx += 1
                                if idx in sig[e]:
                                    ins.then_inc(inc[0], 1)
                return body
            block.sync(run("sp"))
            block.tensor(run("pe"))
            block.scalar(run("act"))
            block.vector(run("dve"))
            block.gpsimd(run("pool"))


def build(NL, taps=(), phases=(2, 31, 32, 33, 41, 42)):
    nc = bass.Bass("TRN2", target_bir_lowering=False)

    def din(name, shape, dt=F32):
        return nc.dram_tensor(name, shape, dt, kind="ExternalInput").ap()

    def dscr(name, shape, dt):
        kind = "ExternalOutput" if name in taps else "Internal"
        return nc.dram_tensor(name, shape, dt, kind=kind).ap()

    x_in = din("x", [S, D])
    w_in = din("w_in", [NL, D, 3584])
    w_out = din("w_out", [NL, D, D])
    w_up = din("w_up", [NL, D, 2 * DFF])
    w_down = din("w_down", [NL, DFF, D])
    ppd = din("pp", [128, NL, NPP])
    pbc = din("pbc", [NL, 2, 128, D])
    cosd = din("cosT", [128, S])
    sind = din("sinT", [128, S])
    cmd = din("cm", [128, 20, 512])
    cstd = din("cst", [128, NCST])
    y = nc.dram_tensor("y", [S, D], F32, kind="ExternalOutput").ap()

    RES = dscr("RES", [S, D], F32)
    QA_T = dscr("QA_T", [2, 128, S], BF16)
    KA_T = dscr("KA_T", [2, 128, S], BF16)
    QC_T = dscr("QC_T", [4, 128, S], BF16)
    KC_T = dscr("KC_T", [4, 128, S], BF16)
    BQ_T = dscr("BQ_T", [2, 2, 128, S], BF16)
    BK_T = dscr("BK_T", [2, 2, 128, S], BF16)
    SG_T = dscr("SG_T", [2, 128, S], BF16)
    TM = dscr("TM", [NT, 128, 1536], BF16)
    EAL = dscr("EAL", [2, 2, 128, 128], F32)
    CC_T = dscr("CC_T", [8, 128, S], BF16)
    HN_T = dscr("HN_T", [8, 128, S + 128], BF16)
    WD = dscr("WD", [22, 128, D], BF16)

    with ExitStack() as es:
        P = Prog(nc, es)
        _ctr = [0]

        from contextlib import contextmanager

        @contextmanager
        def _phase(pid):
            snap = {e: len(P.stream[e]) for e in P.stream}
            cnts = dict(P.cnt)
            dqn = {q: P.dq[q]["n"] for q in P.dq}
            waited = dict(P.waited)
            yield
            if pid not in phases:
                for e in P.stream:
                    del P.stream[e][snap[e]:]
                P.cnt.update(cnts)
                for q in P.dq:
                    P.dq[q]["n"] = dqn[q]
                P.waited.clear()
                P.waited.update(waited)
                for b in P.bufs.values():
                    b.w = None
                    b.rs = []

        def sb(name, shape, dt, st=es):
            _ctr[0] += 1
            return st.enter_context(nc.sbuf_tensor("%s_%d" % (name, _ctr[0]), shape, dt))
        psA = es.enter_context(nc.psum_tensor("psA", [128, 7, 512], F32))
        psT = es.enter_context(nc.psum_tensor("psT", [128, 1024], BF16))
        bank = lambda i: psA[:, i, :]
        psD = psT[:].bitcast(F32)

        def ACT(out, in_, func, reads, writes, **kw):
            return P.op("act", lambda e: e.activation(out=out, in_=in_, func=func, **kw), reads, writes)

        def MM(out, lhsT, rhs, start, stop, reads, writes):
            return P.op("pe", lambda e: e.matmul(out, lhsT=lhsT, rhs=rhs, start=start, stop=stop), reads, writes)

        def TR(out, in_, ident, reads, writes):
            return P.op("pe", lambda e: e.transpose(out=out, in_=in_, identity=ident), reads, writes)

        def TT(eng, out, in0, in1, op, reads, writes):
            return P.op(eng, lambda e: e.tensor_tensor(out=out, in0=in0, in1=in1, op=op), reads, writes)

        def TS(eng, out, in0, s1, s2, op0, op1, reads, writes):
            if s2 is None:
                return P.op(eng, lambda e: e.tensor_scalar(out=out, in0=in0, scalar1=s1, scalar2=None, op0=op0), reads, writes)
            return P.op(eng, lambda e: e.tensor_scalar(out=out, in0=in0, scalar1=s1, scalar2=s2, op0=op0, op1=op1), reads, writes)

        def STT(eng, out, in0, scalar, in1, op0, op1, reads, writes):
            return P.op(eng, lambda e: e.scalar_tensor_tensor(out=out, in0=in0, scalar=scalar, in1=in1, op0=op0, op1=op1), reads, writes)

        def CP(eng, out, in_, reads, writes):
            if eng == "act":
                return P.op("act", lambda e: e.copy(out=out, in_=in_), reads, writes)
            return P.op(eng, lambda e: e.tensor_copy(out=out, in_=in_), reads, writes)

        def tmload(dst, c0, name):
            for g8 in range(8):
                P.dma(dst[:, g8 * 8:(g8 + 1) * 8, :], TM[g8 * 8:(g8 + 1) * 8, :, c0:c0 + 128].rearrange("t p c -> p t c"), writes=[name])

        def rstd_from_ss(rs, ss, n, rname, ssname):
            ACT(rs, ss, AF.Ln, [ssname], [rname], bias=EPS, scale=1.0 / n)
            ACT(rs, rs, AF.Exp, [rname], [rname], scale=-0.5)

        cst = sb("cst", [128, NCST], F32)
        cb = sb("cb", [128, 768], BF16)
        pp = sb("pp", [128, NL, NPP], F32)
        lbs = sb("lbs", [128, 2, 2, 4], F32)
        oml = sb("oml", [128, 2, 2, 4], F32)
        lbt = sb("lbt", [128, 4, 4], F32)
        lbsum = sb("lbsum", [128, 4], F32)
        lam = sb("lam", [128, NL, 4], F32)
        lamt = sb("lamt", [128, 128], F32)
        P.dma(cst[:], cstd[:, :], writes=["cst"])
        P.dma(pp[:], ppd[:, :, :], writes=["pp"])
        CP("dve", cb[:], cst[:, 0:768], ["cst"], ["cb"])
        ident = cb[:, 0:128]
        Rm = cb[:, 128:256]
        ones = cb[:, 256:384]
        bd64 = cb[:, 384:512]
        maskF = cb[:, 512:640]
        maskB = cb[:, 640:768]
        scanm = cst[:, 768:1280]
        lg = pp[:, 0, 192:208].rearrange("p (a l) -> p a l", l=4)
        ACT(lbt[:], lg, AF.Exp, ["pp"], ["lbt"])
        P.op("dve", lambda e: e.tensor_reduce(out=lbsum[:], in_=lbt[:], axis=AX.X, op=ALU.add), ["lbt"], ["lbsum"])
        P.op("dve", lambda e: e.reciprocal(out=lbsum[:], in_=lbsum[:]), ["lbsum"], ["lbsum"])
        TT("dve", lbt[:], lbt[:], lbsum[:].unsqueeze(2).broadcast_to([128, 4, 4]), ALU.mult, ["lbt", "lbsum"], ["lbt"])
        lbs3 = lbs[:].rearrange("p a b l -> p (a b) l")
        oml3 = oml[:].rearrange("p a b l -> p (a b) l")
        P.op("dve", lambda e: e.memset(lbs[:], 0.0), [], ["lbs"])
        for l in range(1, 4):
            TT("dve", lbs3[:, :, l:l + 1], lbs3[:, :, l - 1:l], lbt[:, :, l:l + 1], ALU.add, ["lbs", "lbt"], ["lbs"])
        TS("dve", oml3, lbs3, -1.0, 1.0, ALU.mult, ALU.add, ["lbs"], ["oml"])
        for l in range(NL):
            dl = pp[:, l, 212:468]
            TT("dve", lamt[:, 0:64], dl[:, 0:64], dl[:, 64:128], ALU.mult, ["pp"], ["lamt"])
            TT("dve", lamt[:, 64:128], dl[:, 128:192], dl[:, 192:256], ALU.mult, ["pp"], ["lamt"])
            P.op("dve", lambda e, l=l: e.tensor_reduce(out=lam[:, l, 2:4], in_=lamt[:].rearrange("p (a b) -> p a b", a=2), axis=AX.X, op=ALU.add), ["lamt"], ["lam"])
            ACT(lam[:, l, 2:4], lam[:, l, 2:4], AF.Exp, ["lam"], ["lam"])
            TT("dve", lam[:, l, 0:1], lam[:, l, 3:4], lam[:, l, 2:3], ALU.subtract, ["lam"], ["lam"])
            TT("dve", lam[:, l, 0:1], lam[:, l, 0:1], pp[:, l, 210:211], ALU.subtract, ["lam", "pp"], ["lam"])
            TT("dve", lam[:, l, 1:2], pp[:, l, 209:210], pp[:, l, 211:212], ALU.mult, ["pp"], ["lam"])

        for l in range(NL):
            xsrc = x_in if l == 0 else RES
            xdst = y if l == NL - 1 else RES
            with ExitStack() as ph, _phase(2):
                win = sb("win", [128, 8, 3584], BF16, ph)
                wst = [sb("wst%d" % i, [128, 8, 256], F32, ph) for i in range(2)]
                xt = sb("xt", [128, D], F32, ph)
                junk = sb("junk", [128, D], BF16, ph)
                xs = sb("xs", [128, D], BF16, ph)
                xnT = sb("xnT", [128, 8, 512], BF16, ph)
                cosb = sb("cosb", [128, 512], F32, ph)
                sinb = sb("sinb", [128, 512], F32, ph)
                ofm = sb("ofm", [128, 22, 512], BF16, ph)
                otm = sb("otm", [128, 4, 1536], BF16, ph)
                st1 = sb("st1", [128, 2], F32, ph)
                qraw = sb("qraw", [128, 512], BF16, ph)
                t1 = sb("t1", [128, 512], F32, ph)
                t2 = sb("t2", [128, 512], F32, ph)
                qf = sb("qf", [128, 512], F32, ph)
                ff = sb("ff", [128, 512], F32, ph)
                gg = sb("gg", [128, 512], F32, ph)
                kk = sb("kk", [128, 512], F32, ph)
                pref = sb("pref", [128, 512], F32, ph)
                dd = sb("dd", [128, 512], F32, ph)
                aa = sb("aa", [128, 512], F32, ph)
                ee = sb("ee", [128, 512], F32, ph)
                khT = sb("khT", [128, 512], BF16, ph)
                ealb = sb("ealb", [128, 2, 2, NB, 8], F32, ph)

                for c in range(14):
                    w = wst[c % 2]
                    P.dma(w[:], w_in[l][:, c * 256:(c + 1) * 256].rearrange("(k p) n -> p k n", p=128), writes=["wst%d" % (c % 2)])
                    TT("pool", win[:, :, c * 256:(c + 1) * 256], w[:], pp[:, l, 0:8].unsqueeze(2).broadcast_to([128, 8, 256]),
                       ALU.mult, ["wst%d" % (c % 2), "pp"], ["win"])

                for tb in range(NB):
                    t0 = tb * 512
                    P.dma(cosb[:], cosd[:, t0:t0 + 512], writes=["cosb"])
                    P.dma(sinb[:], sind[:, t0:t0 + 512], writes=["sinb"])
                    for ti in range(4):
                        r0 = t0 + ti * 128
                        P.dma(xt[:], xsrc[r0:r0 + 128, :], writes=["xt"])
                        ACT(junk[:], xt[:], AF.Square, ["xt"], ["junk", "st1"], accum_out=st1[:, 0:1])
                        rstd_from_ss(st1[:, 1:2], st1[:, 0:1], D, "st1", "st1")
                        TS("dve", xs[:], xt[:], st1[:, 1:2], None, ALU.mult, None, ["xt", "st1"], ["xs"])
                        for k in range(8):
                            TR(psT[:, k * 128:(k + 1) * 128], xs[:, k * 128:(k + 1) * 128], ident, ["xs", "cb"], ["bT"])
                        CP("dve", xnT[:, :, ti * 128:(ti + 1) * 128], psT[:].rearrange("p (k t) -> p k t", k=8), ["bT"], ["xnT"])

                    def proj(bi, c0):
                        for k in range(8):
                            MM(bank(bi), win[:, k, c0:c0 + 128], xnT[:, k, :], k == 0, k == 7, ["win", "xnT"], ["b%d" % bi])

                    ropes = [(0, 0, QA_T[0]), (1, 128, QA_T[1]), (2, 256, KA_T[0]), (3, 384, KA_T[1])]
                    for h in range(4):
                        ropes.append((4 + h, 2048 + h * 128, QC_T[h]))
                        ropes.append((8 + h, 2560 + h * 128, KC_T[h]))
                    for ri, (oi, c0, dst) in enumerate(ropes):
                        bi = ri % 2
                        proj(bi, c0)
                        CP("act", qraw[:], bank(bi), ["b%d" % bi], ["qraw"])
                        MM(bank(2 + bi), Rm, qraw[:], True, True, ["qraw", "cb"], ["b%d" % (2 + bi)])
                        TT("dve", t1[:], bank(bi), cosb[:], ALU.mult, ["b%d" % bi, "cosb"], ["t1"])
                        TT("dve", t2[:], bank(2 + bi), sinb[:], ALU.mult, ["b%d" % (2 + bi), "sinb"], ["t2"])
                        TT("dve", ofm[:, oi, :], t1[:], t2[:], ALU.add, ["t1", "t2"], ["ofm%d" % oi])
                        P.dma(dst[:, t0:t0 + 512], ofm[:, oi, :], reads=["ofm%d" % oi])

                    for ti in range(4):
                        for gi, (c0, n, o0) in enumerate([(512, 256, 0), (1536, 256, 256), (3072, 512, 512)]):
                            bi = 4 + (ti * 3 + gi) % 2
                            for k in range(8):
                                MM(bank(bi)[:, 0:n], xnT[:, k, ti * 128:(ti + 1) * 128], win[:, k, c0:c0 + n], k == 0, k == 7,
                                   ["xnT", "win"], ["b%d" % bi])
                            CP("act", otm[:, ti, o0:o0 + n], bank(bi)[:, 0:n], ["b%d" % bi], ["otmv%d" % ti])

                    for p in range(2):
                        proj(0, 1792 + p * 128)
                        ACT(ofm[:, 20 + p, :], bank(0), AF.Silu, ["b0"], ["ofm%d" % (20 + p)])
                        P.dma(SG_T[p][:, t0:t0 + 512], ofm[:, 20 + p, :], reads=["ofm%d" % (20 + p)])
                        proj(1, 768 + p * 128)
                        CP("act", qf[:], bank(1), ["b1"], ["qf"])
                        for di in range(2):
                            proj(2, (1024 if di == 0 else 1280) + p * 128)
                            ACT(ff[:], bank(2), AF.Sigmoid, ["b2"], ["ff"])
                            TS("dve", ff[:], ff[:], oml[:, di, p, l:l + 1], lbs[:, di, p, l:l + 1], ALU.mult, ALU.add, ["ff", "oml", "lbs"], ["ff"])
                            ACT(gg[:], ff[:], AF.Ln, ["ff"], ["gg"])
                            TS("dve", kk[:], ff[:], -1.0, 1.0, ALU.mult, ALU.add, ["ff"], ["kk"])
                            P.op("dve", lambda e: e.tensor_tensor_scan(out=pref[:], data0=scanm, data1=gg[:], initial=0.0, op0=ALU.mult, op1=ALU.add),
                                 ["gg", "cst"], ["pref"])
                            pref3 = pref[:].rearrange("p (c t) -> p c t", t=64)
                            tot = pref3[:, :, 63:64]
                            TT("dve", dd[:].rearrange("p (c t) -> p c t", t=64), tot.broadcast_to([128, 8, 64]), pref3, ALU.subtract, ["pref"], ["dd"])
                            if di == 0:
                                a_ap, a_nm = pref, "pref"
                                dk_ap, dk_nm = dd, "dd"
                            else:
                                TT("dve", aa[:], dd[:], gg[:], ALU.add, ["dd", "gg"], ["aa"])
                                TT("dve", dd[:], pref[:], gg[:], ALU.subtract, ["pref", "gg"], ["dd"])
                                a_ap, a_nm = aa, "aa"
                                dk_ap, dk_nm = dd, "dd"
                            oq = 12 + p * 4 + di * 2
                            ACT(ee[:], a_ap[:], AF.Exp, [a_nm], ["ee"])
                            TT("dve", ofm[:, oq, :], qf[:], ee[:], ALU.mult, ["qf", "ee"], ["ofm%d" % oq])
                            P.dma(BQ_T[p, di][:, t0:t0 + 512], ofm[:, oq, :], reads=["ofm%d" % oq])
                            ACT(ee[:], a_ap[:], AF.Exp, [a_nm], ["ee"], scale=-1.0)
                            TT("dve", ofm[:, oq + 1, :], kk[:], ee[:], ALU.mult, ["kk", "ee"], ["ofm%d" % (oq + 1)])
                            P.dma(BK_T[p, di][:, t0:t0 + 512], ofm[:, oq + 1, :], reads=["ofm%d" % (oq + 1)])
                            ACT(ee[:], dk_ap[:], AF.Exp, [dk_nm], ["ee"])
                            TT("dve", khT[:], kk[:], ee[:], ALU.mult, ["kk", "ee"], ["khT"])
                            for ti in range(4):
                                TR(psT[:, ti * 128:(ti + 1) * 128], khT[:, ti * 128:(ti + 1) * 128], ident, ["khT", "cb"], ["bT"])
                            o0 = 1024 + di * 256 + p * 128
                            CP("dve", otm[:, :, o0:o0 + 128], psT[:, 0:512].rearrange("p (t c) -> p t c", t=4), ["bT"], ["otmk"])
                            ACT(ealb[:, p, di, tb, :], tot.rearrange("p c o -> p (c o)"), AF.Exp, ["pref"], ["ealb"])
                    for ti in range(4):
                        P.dma(TM[tb * 4 + ti], otm[:, ti, :], reads=["otmv%d" % ti, "otmk"])
                for p in range(2):
                    for di in range(2):
                        P.dma(EAL[p, di], ealb[:, p, di].rearrange("p b c -> p (b c)"), reads=["ealb"])
                P.barrier()

            with ExitStack() as ph, _phase(31):
                cmf = sb("cmf", [128, 4, 512], F32, ph)
                cmb = sb("cmb", [128, 20, 512], BF16, ph)
                qT = sb("qT", [128, S], BF16, ph)
                kT = sb("kT", [128, S], BF16, ph)
                vv = sb("vv", [128, NT, 128], BF16, ph)
                pT = [sb("pT%d" % i, [128, 512], BF16, ph) for i in range(3)]
                pm = [sb("pm%d" % i, [128, 512], BF16, ph) for i in range(3)]
                rden = sb("rden", [128, 512], F32, ph)
                osb = [sb("osb%d" % i, [128, 512], BF16, ph) for i in range(2)]
                for c in range(5):
                    P.dma(cmf[:], cmd[:, c * 4:(c + 1) * 4, :], writes=["cmf"])
                    CP("dve", cmb[:, c * 4:(c + 1) * 4, :], cmf[:], ["cmf"], ["cmb"])
                for p in range(2):
                    P.dma(qT[:], QA_T[p], writes=["qT"])
                    P.dma(kT[:], KA_T[p], writes=["kT"])
                    tmload(vv, p * 128, "vv")
                    units = []
                    for qb in range(NB):
                        for h in range(2):
                            kcs = [kc for kc in range(4 * qb - 8, 4 * qb + 12) if 0 <= kc < NT]
                            for j, kc in enumerate(kcs):
                                units.append((qb, h, kc, j == 0, j == len(kcs) - 1))

                    def smm(idx):
                        qb, h, kc, first, last = units[idx]
                        hr = slice(h * 64, (h + 1) * 64)
                        bi = idx % 3
                        MM(bank(bi), kT[hr, kc * 128:(kc + 1) * 128], qT[hr, qb * 512:(qb + 1) * 512], True, True, ["kT", "qT"], ["b%d" % bi])

                    smm(0)
                    smm(1)
                    for idx, (qb, h, kc, first, last) in enumerate(units):
                        if idx + 2 < len(units):
                            smm(idx + 2)
                        hr = slice(h * 64, (h + 1) * 64)
                        bi = idx % 3
                        rel = kc - 4 * qb + 8
                        ob = osb[qb % 2]
                        obn = "osb%d" % (qb % 2)
                        ACT(pT[bi][:], bank(bi), AF.Exp, ["b%d" % bi], ["pT%d" % bi], scale=0.125)
                        TT("dve", pm[bi][:], pT[bi][:], cmb[:, rel, :], ALU.mult, ["pT%d" % bi, "cmb"], ["pm%d" % bi])
                        MM(bank(3), vv[:, kc, :], pm[bi][:], first, last, ["vv", "pm%d" % bi], ["b3"])
                        MM(bank(4), ones, pm[bi][:], first, last, ["cb", "pm%d" % bi], ["b4"])
                        if last:
                            P.op("dve", lambda e, hr=hr: e.reciprocal(out=rden[hr, :], in_=bank(4)[hr, :]), ["b4"], ["rden"])
                            TT("dve", ob[hr, :], bank(3)[hr, :], rden[hr, :], ALU.mult, ["b3", "rden"], [obn])
                            if h == 1:
                                P.dma(CC_T[p][:, qb * 512:(qb + 1) * 512], ob[:], reads=[obn])
                P.barrier()

            with ExitStack() as ph, _phase(32):
                qT = sb("hqT", [128, S], BF16, ph)
                kT = sb("hkT", [128, S], BF16, ph)
                kh = sb("hkh", [128, NT, 128], BF16, ph)
                vv = sb("hvv", [128, NT, 128], BF16, ph)
                obuf = sb("obuf", [128, S], F32, ph)
                eal = sb("eal", [128, 128], F32, ph)
                Sf = sb("Sf", [128, 128], F32, ph)
                Sb = sb("Sb", [128, 128], BF16, ph)
                sT = [sb("sT%d" % i, [128, 2, 128], BF16, ph) for i in range(2)]
                mD = sb("mD", [128, 2, 2, 128], BF16, ph)
                sq = sb("sq", [128, 512], BF16, ph)
                rs = sb("rs", [128, 512], F32, ph)
                tq = sb("tq", [128, 512], F32, ph)
                sgb = [sb("sgb%d" % i, [128, 512], BF16, ph) for i in range(2)]
                ocb = [sb("ocb%d" % i, [128, 512], BF16, ph) for i in range(2)]
                P.op("dve", lambda e: e.memset(mD[:], 0.0), [], ["mD"])
                for di, mk in enumerate((maskF, maskB)):
                    for h in range(2):
                        CP("dve", mD[0:64, di, h, 0:64], mk[0:64, 0:64], ["cb", "mD"], ["mD"])
                        CP("dve", mD[64:128, di, h, 64:128], mk[64:128, 64:128], ["cb", "mD"], ["mD"])
                for p in range(2):
                    tmload(vv, 256 + p * 128, "hvv")
                    for di in range(2):
                        P.dma(qT[:], BQ_T[p, di], writes=["hqT"])
                        P.dma(kT[:], BK_T[p, di], writes=["hkT"])
                        o0 = 1024 + di * 256 + p * 128
                        tmload(kh, o0, "hkh")
                        P.dma(eal[:], EAL[p, di], writes=["eal"])
                        P.op("dve", lambda e: e.memset(Sf[:], 0.0), [], ["Sf"])
                        P.op("dve", lambda e: e.memset(Sb[:], 0.0), [], ["Sb"])
                        tiles = range(NT) if di == 0 else range(NT - 1, -1, -1)
                        for n, ti in enumerate(tiles):
                            tc0 = ti * 128
                            sb_i = n % 2
                            for h in range(2):
                                hr = slice(h * 64, (h + 1) * 64)
                                sbk, sbn = (bank(sb_i), "b%d" % sb_i) if h == 0 else (bank(6), "b6")
                                MM(sbk[:, 0:128], kT[hr, tc0:tc0 + 128], qT[hr, tc0:tc0 + 128], True, True, ["hkT", "hqT"], [sbn])
                                TT("dve", sT[sb_i][:, h, :], sbk[:, 0:128], mD[:, di, h, :], ALU.mult, [sbn, "mD"], ["sT%d" % sb_i])
                            ob_i = 2 + n % 2
                            for cc in ((0, 1) if di == 0 else (1, 0)):
                                c = ti * 2 + cc
                                cs = slice(cc * 64, (cc + 1) * 64)
                                for h in range(2):
                                    hr = slice(h * 64, (h + 1) * 64)
                                    oo = bank(ob_i)[:, h * 128 + cc * 64:h * 128 + (cc + 1) * 64]
                                    MM(oo, vv[:, ti, :], sT[sb_i][:, h, cs], True, False, ["hvv", "sT%d" % sb_i], ["b%d" % ob_i])
                                    MM(oo, Sb[hr, :], qT[hr, tc0 + cc * 64:tc0 + (cc + 1) * 64], False, True, ["Sb", "hqT"], ["b%d" % ob_i])
                                MM(bank(4)[:, 0:128], kh[cs, ti, :], vv[cs, ti, :], True, True, ["hkh", "hvv"], ["b4"])
                                STT("dve", Sf[:], Sf[:], eal[:, c:c + 1], bank(4)[:, 0:128], ALU.mult, ALU.add, ["Sf", "eal", "b4"], ["Sf"])
                                CP("act", Sb[:], Sf[:], ["Sf"], ["Sb"])
                            for h in range(2):
                                hr = slice(h * 64, (h + 1) * 64)
                                src = bank(ob_i)[hr, h * 128:(h + 1) * 128]
                                if di == 0:
                                    CP("act", obuf[hr, tc0:tc0 + 128], src, ["b%d" % ob_i], ["obuf%d" % ti])
                                else:
                                    TT("dve", obuf[hr, tc0:tc0 + 128], obuf[hr, tc0:tc0 + 128], src, ALU.add, ["b%d" % ob_i, "obuf%d" % ti], ["obuf%d" % ti])
                    for tb in range(NB):
                        t0 = tb * 512
                        sl = slice(t0, t0 + 512)
                        rn = ["obuf%d" % (tb * 4 + i) for i in range(4)]
                        i2 = tb % 2
                        P.dma(sgb[i2][:], SG_T[p][:, sl], writes=["sgb%d" % i2])
                        ACT(sq[:], obuf[:, sl], AF.Square, rn, ["sq"])
                        MM(bank(5), bd64, sq[:], True, True, ["cb", "sq"], ["b5"])
                        rstd_from_ss(rs[:], bank(5), 64, "rs", "b5")
                        TT("dve", tq[:], obuf[:, sl], rs[:], ALU.mult, rn + ["rs"], ["tq"])
                        STT("dve", ocb[i2][:], tq[:], pp[:, l, 208:209], sgb[i2][:], ALU.mult, ALU.mult, ["tq", "pp", "sgb%d" % i2], ["ocb%d" % i2])
                        P.dma(CC_T[2 + p][:, sl], ocb[i2][:], reads=["ocb%d" % i2])
                P.barrier()

            with ExitStack() as ph, _phase(33):
                qT = sb("cqT", [128, S], BF16, ph)
                kT = sb("ckT", [128, S], BF16, ph)
                vv = sb("cvv", [128, NT, 128], BF16, ph)
                pT = [sb("cpT%d" % i, [128, 2, 512], BF16, ph) for i in range(2)]
                r1 = sb("r1", [128, 512], F32, ph)
                r2 = sb("r2", [128, 512], F32, ph)
                u1 = sb("u1", [128, 512], F32, ph)
                u2 = sb("u2", [128, 512], F32, ph)
                sq = sb("csq", [128, 512], BF16, ph)
                rs = sb("crs", [128, 512], F32, ph)
                ocb = [sb("cocb%d" % i, [128, 512], BF16, ph) for i in range(2)]
                acc = [sb("acc%d" % i, [128, 2, 512], F32, ph) for i in range(2)]
                onesf = cst[:, 256:384]
                for h in range(4):
                    P.dma(qT[:], QC_T[h], writes=["cqT"])
                    P.dma(kT[:], KC_T[h], writes=["ckT"])
                    tmload(vv, 512 + h * 128, "cvv")
                    units = [(qb, kc) for qb in range(NB) for kc in range(NT)]

                    def qk(idx):
                        qb, kc = units[idx]
                        i2 = idx % 2
                        for m in range(2):
                            mr = slice(m * 64, (m + 1) * 64)
                            MM(bank(2 * i2 + m), kT[mr, kc * 128:(kc + 1) * 128], qT[mr, qb * 512:(qb + 1) * 512], True, True, ["ckT", "cqT"], ["s%d" % i2])

                    qk(0)
                    for idx, (qb, kc) in enumerate(units):
                        if idx + 1 < len(units):
                            qk(idx + 1)
                        i2 = idx % 2
                        qs = slice(qb * 512, (qb + 1) * 512)
                        ACT(pT[i2][:], psA[:, 2 * i2:2 * i2 + 2, :], AF.Exp, ["s%d" % i2], ["cpT%d" % i2], scale=0.125)
                        for m in range(2):
                            MM(bank(4 + m), vv[:, kc, :], pT[i2][:, m, :], kc == 0, kc == NT - 1, ["cvv", "cpT%d" % i2], ["o%d" % m])
                        a2 = qb % 2
                        if kc == 0:
                            CP("dve", acc[a2][:], pT[i2][:], ["cpT%d" % i2], ["acc%d" % a2])
                        else:
                            TT("dve", acc[a2][:], acc[a2][:], pT[i2][:], ALU.add, ["acc%d" % a2, "cpT%d" % i2], ["acc%d" % a2])
                        if kc == NT - 1:
                            MM(bank(6), onesf, acc[a2][:, 0, :], True, True, ["cst", "acc%d" % a2], ["d0"])
                            MM(psD, onesf, acc[a2][:, 1, :], True, True, ["cst", "acc%d" % a2], ["d1"])
                            P.op("dve", lambda e: e.reciprocal(out=r1[:], in_=bank(6)), ["d0"], ["r1"])
                            P.op("dve", lambda e: e.reciprocal(out=r2[:], in_=psD), ["d1"], ["r2"])
                            TT("dve", u1[:], bank(4), r1[:], ALU.mult, ["o0", "r1"], ["u1"])
                            TT("dve", u2[:], bank(5), r2[:], ALU.mult, ["o1", "r2"], ["u2"])
                            STT("dve", u1[:], u2[:], lam[:, l, 0:1], u1[:], ALU.mult, ALU.add, ["u1", "u2", "lam"], ["u1"])
                            ACT(sq[:], u1[:], AF.Square, ["u1"], ["csq"])
                            MM(bank(6), ones, sq[:], True, True, ["cb", "csq"], ["d0"])
                            rstd_from_ss(rs[:], bank(6), 128, "crs", "d0")
                            o2 = qb % 2
                            TT("dve", u2[:], u1[:], rs[:], ALU.mult, ["u1", "crs"], ["u2"])
                            TS("dve", ocb[o2][:], u2[:], lam[:, l, 1:2], None, ALU.mult, None, ["u2", "lam"], ["cocb%d" % o2])
                            P.dma(CC_T[4 + h][:, qs], ocb[o2][:], reads=["cocb%d" % o2])
                P.barrier()

            with ExitStack() as ph, _phase(41):
                wo = sb("wo", [128, 8, D], BF16, ph)
                wst = [sb("wst%d" % i, [128, 8, 256], F32, ph) for i in range(2)]
                ccb = [sb("ccb%d" % i, [128, 8, 512], BF16, ph) for i in range(2)]
                xt = [sb("xt%d" % i, [128, D], F32, ph) for i in range(2)]
                gpo = sb("gpo", [128, D], F32, ph)
                tm = sb("tm", [128, D], F32, ph)
                hh = [sb("hh%d" % i, [128, D], F32, ph) for i in range(2)]
                junk = sb("junk", [128, D], BF16, ph)
                hs = sb("hs", [128, D], BF16, ph)
                hT = [sb("hT%d" % i, [128, 8, 512], BF16, ph) for i in range(2)]
                st1 = sb("st1", [128, 4], F32, ph)
                zz = sb("zz", [128, 8, 64], BF16, ph)
                P.op("dve", lambda e: e.memset(zz[:], 0.0), [], ["zz"])
                P.dma(HN_T[:, :, 0:64].rearrange("k p o -> p k o"), zz[:], reads=["zz"])
                P.dma(HN_T[:, :, S + 64:S + 128].rearrange("k p o -> p k o"), zz[:], reads=["zz"])
                P.dma(gpo[:], pbc[l, 0], writes=["gpo"])
                for c in range(4):
                    w = wst[c % 2]
                    P.dma(w[:], w_out[l][:, c * 256:(c + 1) * 256].rearrange("(k p) n -> p k n", p=128), writes=["wst%d" % (c % 2)])
                    CP("pool", wo[:, :, c * 256:(c + 1) * 256], w[:], ["wst%d" % (c % 2)], ["wo"])
                for tb in range(NB):
                    t0 = tb * 512
                    b2 = tb % 2
                    P.dma(ccb[b2][:], CC_T[:, :, t0:t0 + 512].rearrange("k p t -> p k t"), writes=["ccb%d" % b2])
                    for ti in range(4):
                        r0 = t0 + ti * 128
                        n = tb * 4 + ti
                        i2 = n % 2
                        P.dma(xt[i2][:], xsrc[r0:r0 + 128, :], writes=["xt%d" % i2])
                        pb = 2 * i2
                        for half in range(2):
                            for k in range(8):
                                MM(bank(pb + half), ccb[b2][:, k, ti * 128:(ti + 1) * 128], wo[:, k, half * 512:(half + 1) * 512], k == 0, k == 7,
                                   ["ccb%d" % b2, "wo"], ["m%d" % i2])
                        mix = psA[:, pb:pb + 2, :].rearrange("p a b -> p (a b)")
                        ACT(junk[:], mix, AF.Square, ["m%d" % i2], ["junk", "st1"], accum_out=st1[:, 0:1])
                        rstd_from_ss(st1[:, 1:2], st1[:, 0:1], D, "st1", "st1")
                        STT("dve", tm[:], mix, st1[:, 1:2], gpo[:], ALU.mult, ALU.mult, ["m%d" % i2, "st1", "gpo"], ["tm"])
                        TT("dve", hh[i2][:], tm[:], xt[i2][:], ALU.add, ["tm", "xt%d" % i2], ["hh%d" % i2])
                        P.dma(RES[r0:r0 + 128, :], hh[i2][:], reads=["hh%d" % i2])
                        ACT(junk[:], hh[i2][:], AF.Square, ["hh%d" % i2], ["junk", "st1"], accum_out=st1[:, 2:3])
                        rstd_from_ss(st1[:, 3:4], st1[:, 2:3], D, "st1", "st1")
                        TS("dve", hs[:], hh[i2][:], st1[:, 3:4], None, ALU.mult, None, ["hh%d" % i2, "st1"], ["hs"])
                        for k in range(8):
                            TR(psT[:, k * 128:(k + 1) * 128], hs[:, k * 128:(k + 1) * 128], ident, ["hs", "cb"], ["bT"])
                        CP("dve", hT[b2][:, :, ti * 128:(ti + 1) * 128], psT[:].rearrange("p (k t) -> p k t", k=8), ["bT"], ["hT%d" % b2])
                    P.dma(HN_T[:, :, 64 + t0:64 + t0 + 512].rearrange("k p t -> p k t"), hT[b2][:], reads=["hT%d" % b2])
                P.barrier()

            with ExitStack() as ph, _phase(42):
                wup = sb("wup", [128, 8, 2 * DFF], BF16, ph)
                wst = [sb("wst%d" % i, [128, 8, 256], F32, ph) for i in range(2)]
                wdb = [sb("wdb%d" % i, [128, D], BF16, ph) for i in range(4)]
                hnT = [sb("hnT%d" % i, [128, 8, 514], BF16, ph) for i in range(2)]
                U = [sb("U%d" % i, [128, 514], F32, ph) for i in range(2)]
                cv = [sb("cv%d" % i, [128, 512], F32, ph) for i in range(2)]
                sgl = sb("sgl", [128, 512], F32, ph)
                actT = sb("actT", [128, 22, 512], BF16, ph)
                hh = [sb("hh%d" % i, [128, D], F32, ph) for i in range(2)]
                gpo = sb("gpo", [128, D], F32, ph)
                oo = [sb("oo%d" % i, [128, D], F32, ph) for i in range(2)]
                junk = sb("junk", [128, D], BF16, ph)
                st1 = sb("st1", [128, 2], F32, ph)
                P.dma(gpo[:], pbc[l, 1], writes=["gpo"])
                for j in range(22):
                    w = wst[j % 2]
                    wv = w[:].rearrange("p k n -> p (k n)")[:, 0:D]
                    P.dma(wv, w_down[l][j * 128:(j + 1) * 128, :], writes=["wst%d" % (j % 2)])
                    CP("pool", wdb[j % 4][:], wv, ["wst%d" % (j % 2)], ["wdb%d" % (j % 4)])
                    P.dma(WD[j], wdb[j % 4][:], reads=["wdb%d" % (j % 4)], writes=["WD"])
                for c in range(22):
                    w = wst[c % 2]
                    P.dma(w[:], w_up[l][:, c * 256:(c + 1) * 256].rearrange("(k p) n -> p k n", p=128), writes=["wst%d" % (c % 2)])
                    TT("pool", wup[:, :, c * 256:(c + 1) * 256], w[:], pp[:, l, 8:16].unsqueeze(2).broadcast_to([128, 8, 256]),
                       ALU.mult, ["wst%d" % (c % 2), "pp"], ["wup"])
                wdi = 0
                for tb in range(NB):
                    t0 = tb * 512
                    b2 = tb % 2
                    hn = hnT[b2]
                    hnn = "hnT%d" % b2
                    P.dma(hn[:], HN_T[:, :, 63 + t0:63 + t0 + 514].rearrange("k p t -> p k t"), writes=[hnn])
                    halo = hn[:, :, 0:514:513]
                    for j in range(22):
                        for gv in range(2):
                            cidx = j + 22 * gv
                            c0 = cidx * 128
                            bi = gv
                            for k in range(8):
                                MM(bank(bi), wup[:, k, c0:c0 + 128], hn[:, k, 1:513], k == 0, k == 7, ["wup", hnn], ["b%d" % bi])
                            for k in range(8):
                                MM(bank(6)[:, gv * 2:gv * 2 + 2], wup[:, k, c0:c0 + 128], hn[:, k, 0:514:513], k == 0, k == 7, ["wup", hnn], ["h6"])
                            Ub = U[gv]
                            un = "U%d" % gv
                            CP("act", Ub[:, 1:513], bank(bi), ["b%d" % bi], [un])
                            CP("act", Ub[:, 0:514:513], bank(6)[:, gv * 2:gv * 2 + 2], ["h6"], [un])
                            cw = lambda r: pp[:, l, 16 + r * 44 + cidx:16 + r * 44 + cidx + 1]
                            cbias = pp[:, l, 148 + cidx:148 + cidx + 1]
                            TS("dve", cv[gv][:], Ub[:, 1:513], cw(1), cbias, ALU.mult, ALU.add, [un, "pp"], ["cv%d" % gv])
                            STT("dve", cv[gv][:], Ub[:, 0:512], cw(0), cv[gv][:], ALU.mult, ALU.add, [un, "pp", "cv%d" % gv], ["cv%d" % gv])
                            STT("dve", cv[gv][:], Ub[:, 2:514], cw(2), cv[gv][:], ALU.mult, ALU.add, [un, "pp", "cv%d" % gv], ["cv%d" % gv])
                        ACT(sgl[:], cv[0][:], AF.Silu, ["cv0"], ["sgl"])
                        TT("dve", actT[:, j, :], sgl[:], cv[1][:], ALU.mult, ["sgl", "cv1"], ["actT"])
                    for ps_ in range(2):
                        for j in range(22):
                            wb = wdb[wdi % 4]
                            wbn = "wdb%d" % (wdi % 4)
                            wdi += 1
                            P.dma(wb[:], WD[j], reads=["WD"], writes=[wbn])
                            for tt_ in range(2):
                                ti = ps_ * 2 + tt_
                                for half in range(2):
                                    MM(bank(2 + tt_ * 2 + half), actT[:, j, ti * 128:(ti + 1) * 128], wb[:, half * 512:(half + 1) * 512],
                                       j == 0, j == 21, ["actT", wbn], ["f%d" % tt_])
                        for tt_ in range(2):
                            ti = ps_ * 2 + tt_
                            r0 = t0 + ti * 128
                            i2 = tt_
                            P.dma(hh[i2][:], RES[r0:r0 + 128, :], writes=["hh%d" % i2])
                            fo = psA[:, 2 + tt_ * 2:4 + tt_ * 2, :].rearrange("p a b -> p (a b)")
                            ACT(junk[:], fo, AF.Square, ["f%d" % tt_], ["junk", "st1"], accum_out=st1[:, 0:1])
                            rstd_from_ss(st1[:, 1:2], st1[:, 0:1], D, "st1", "st1")
                            STT("dve", oo[i2][:], fo, st1[:, 1:2], gpo[:], ALU.mult, ALU.mult, ["f%d" % tt_, "st1", "gpo"], ["oo%d" % i2])
                            TT("dve", oo[i2][:], oo[i2][:], hh[i2][:], ALU.add, ["oo%d" % i2, "hh%d" % i2], ["oo%d" % i2])
                            P.dma(xdst[r0:r0 + 128, :], oo[i2][:], reads=["oo%d" % i2])
                P.barrier()
        P.emit()
    return nc


def _consts():
    pos = np.arange(S, dtype=np.float32)
    inv = (np.float32(500000.0) ** (-np.arange(0, 16, 2, dtype=np.float32) / np.float32(16))).astype(np.float32)
    ang = pos[None, :] * inv[:, None]
    cosT = np.ones((128, S), np.float32)
    sinT = np.zeros((128, S), np.float32)
    for p in range(128):
        j = p % 64
        if j < 8:
            cosT[p] = np.cos(ang[j]); sinT[p] = -np.sin(ang[j])
        elif j < 16:
            cosT[p] = np.cos(ang[j - 8]); sinT[p] = np.sin(ang[j - 8])
    cm = np.zeros((128, 20, 512), np.float32)
    i = np.arange(128)[:, None]
    jq = np.arange(512)[None, :]
    for rel in range(20):
        d = (rel - 8) * 128 + i - jq
        ad = np.abs(d)
        cm[:, rel, :] = (ad <= 64).astype(np.float32) + ((d % 4 == 0) & (ad <= 256)) + ((d % 16 == 0) & (ad <= 1024))
    cst = np.zeros((128, NCST), np.float32)
    cst[:, 0:128] = np.eye(128)
    Rm = np.zeros((128, 128), np.float32)
    for m in range(128):
        j = m % 64
        if j < 8:
            Rm[m + 8, m] = 1.0
        elif j < 16:
            Rm[m - 8, m] = 1.0
    cst[:, 128:256] = Rm
    cst[:, 256:384] = 1.0
    bd = np.zeros((128, 128), np.float32)
    bd[0:64, 0:64] = 1.0
    bd[64:128, 64:128] = 1.0
    cst[:, 384:512] = bd
    s_ = np.arange(128)[:, None] % 64
    t_ = np.arange(128)[None, :] % 64
    cst[:, 512:640] = (s_ <= t_)
    cst[:, 640:768] = (s_ >= t_)
    m = np.ones(512, np.float32)
    m[0::64] = 0.0
    cst[:, 768:1280] = m[None, :]
    return cosT, sinT, cm, cst


def _pack_params(inp, layers):
    NL = len(layers)
    pp = np.zeros((128, NL, NPP), np.float32)
    pbc = np.zeros((NL, 2, 128, D), np.float32)
    lb = inp["lb_logits"]
    lbl = np.transpose(lb.reshape(2, 4, 2, 128), (3, 0, 2, 1)).reshape(128, 16)
    for i, l in enumerate(layers):
        pp[:, i, 0:8] = inp["norm_pre_mix"][l].reshape(8, 128).T
        pp[:, i, 8:16] = inp["norm_pre_ffn"][l].reshape(8, 128).T
        pp[:, i, 16:148] = np.transpose(inp["conv_w"][l].reshape(3, 44, 128), (2, 0, 1)).reshape(128, 132)
        pp[:, i, 148:192] = inp["conv_b"][l].reshape(44, 128).T
        pp[:, i, 192:208] = lbl
        pp[:, i, 208] = np.tile(inp["hgrn_norm"][l], 2)
        pp[:, i, 209] = inp["diff_norm"][l]
        lam_init = 0.8 - 0.6 * math.exp(-0.3 * l)
        pp[:, i, 210] = lam_init
        pp[:, i, 211] = 1.0 - lam_init
        pp[:, i, 212:468] = np.broadcast_to(inp["diff_lambda"][l].reshape(1, 256), (128, 256))
        pbc[i, 0] = np.broadcast_to(inp["norm_post_mix"][l][None, :], (128, D))
        pbc[i, 1] = np.broadcast_to(inp["norm_post_ffn"][l][None, :], (128, D))
    return pp, pbc


_NC_CACHE = {}


def kernel(x, w_in, w_out, lb_logits, hgrn_norm, diff_lambda, diff_norm, w_up, conv_w, conv_b,
           w_down, norm_pre_mix, norm_post_mix, norm_pre_ffn, norm_post_ffn):
    inp = dict(x=x, w_in=w_in, w_out=w_out, lb_logits=lb_logits, hgrn_norm=hgrn_norm, diff_lambda=diff_lambda,
               diff_norm=diff_norm, w_up=w_up, conv_w=conv_w, conv_b=conv_b, w_down=w_down, norm_pre_mix=norm_pre_mix,
               norm_post_mix=norm_post_mix, norm_pre_ffn=norm_pre_ffn, norm_post_ffn=norm_post_ffn)
    inp = {k: np.ascontiguousarray(np.asarray(v, dtype=np.float32)) for k, v in inp.items()}
    NL = 4
    if NL not in _NC_CACHE:
        _NC_CACHE[NL] = build(NL)
    nc = _NC_CACHE[NL]
    cosT, sinT, cm, cst = _consts()
    pp, pbc = _pack_params(inp, list(range(NL)))
    in_maps = []
    for c in range(8):
        b = c // 2
        in_maps.append({"x": inp["x"][b], "w_in": inp["w_in"], "w_out": inp["w_out"], "w_up": inp["w_up"], "w_down": inp["w_down"],
                        "pp": pp, "pbc": pbc, "cosT": cosT, "sinT": sinT, "cm": cm, "cst": cst})
    res = run_bass_kernel_spmd(nc, in_maps, core_ids=list(range(8)))
    out = np.stack([np.asarray(res.results[2 * b]["y"], dtype=np.float32) for b in range(4)], axis=0)
    return out
```

```python
import math
import numpy as np
import concourse.bass as bass
import concourse.mybir as mybir
from concourse.bass_utils import run_bass_kernel_spmd
from contextlib import ExitStack

F32 = mybir.dt.float32
BF16 = mybir.dt.bfloat16
AF = mybir.ActivationFunctionType
ALU = mybir.AluOpType
AX = mybir.AxisListType

S = 8192
D = 1024
NT = S // 128
NB = S // 512
DFF = 2816
NPP = 468
NCST = 1280
EPS = 1e-6


def _is_psum(name):
    return name == "bT" or (len(name) == 2 and name[0] in "bsodmfh" and name[1].isdigit())


class Buf:
    __slots__ = ("name", "w", "rs")

    def __init__(self, name):
        self.name = name
        self.w = None
        self.rs = []


class Prog:
    NDMA = 8

    def __init__(self, nc, es):
        self.nc = nc
        self.sem = {e: es.enter_context(nc.semaphore("c_" + e)) for e in ("pe", "act", "dve", "pool")}
        self.cnt = {e: 0 for e in self.sem}
        self.dq = {}
        for q in ("sp", "act", "pool"):
            self.dq[q] = dict(sems=[es.enter_context(nc.semaphore("d_%s%d" % (q, i))) for i in range(self.NDMA)], n=0)
        self.stream = {e: [] for e in ("pe", "act", "dve", "pool", "sp")}
        self.waited = {}
        self.bufs = {}
        import os
        self.maxops = int(os.environ.get("KMAXOPS", "0"))
        self.nops = 0

    def buf(self, name):
        b = self.bufs.get(name)
        if b is None:
            b = self.bufs[name] = Buf(name)
        return b

    def _need(self, e, toks):
        out = {}
        for t in toks:
            if t is None:
                continue
            sem, val, _ = t
            if self.waited.get((e, id(sem)), 0) >= val:
                continue
            if id(sem) not in out or out[id(sem)][1] < val:
                out[id(sem)] = (sem, val)
        for sid, (sem, val) in out.items():
            self.waited[(e, sid)] = val
        return list(out.values())

    def _deps(self, reads, writes):
        toks = []
        for b in reads:
            toks.append(b.w)
        for b in writes:
            toks.append(b.w)
            toks.extend(b.rs)
        return toks

    def _commit(self, tok, reads, writes):
        for b in reads:
            b.rs.append(tok)
            if len(b.rs) > 64:
                b.rs = b.rs[-64:] if False else b.rs
        for b in writes:
            b.w = tok
            b.rs = []

    def op(self, e, fn, reads=(), writes=()):
        self.nops += 1
        if self.maxops:
            import sys
            f = sys._getframe(1)
            f2 = f.f_back
            print("OP", self.nops, e, f.f_code.co_name, f.f_lineno, f2.f_lineno, list(reads), list(writes))
        if self.maxops and self.nops > self.maxops:
            return None
        writes = list(writes) + [b for b in reads if _is_psum(b)]
        reads = [self.buf(b) for b in reads if not _is_psum(b)]
        writes = [self.buf(b) for b in writes]
        toks = self._deps(reads, writes)
        if e == "pe":
            toks = [t for t in toks if t is not None and t[2] != "pe"]
        waits = self._need(e, toks)
        self.cnt[e] += 1
        tok = (self.sem[e], self.cnt[e], e)
        self.stream[e].append((waits, fn, (self.sem[e], 1)))
        self._commit(tok, reads, writes)
        return tok

    def dma(self, out, in_, reads=(), writes=(), q="sp"):
        self.nops += 1
        if self.maxops:
            import sys
            f = sys._getframe(1)
            print("OP", self.nops, "dma", f.f_lineno, list(reads), list(writes))
        if self.maxops and self.nops > self.maxops:
            return None
        reads = [self.buf(b) for b in reads]
        writes = [self.buf(b) for b in writes]
        d = self.dq[q]
        i = d["n"]
        d["n"] += 1
        sem = d["sems"][i % self.NDMA]
        rnd = i // self.NDMA
        toks = self._deps(reads, writes)
        if rnd > 0:
            toks.append((sem, 16 * rnd, q))
        waits = self._need(q, toks)
        tok = (sem, 16 * (rnd + 1), q)
        self.stream[q].append((waits, (lambda eng, out=out, in_=in_: eng.dma_start(out=out, in_=in_)), (sem, 16)))
        self._commit(tok, reads, writes)
        return tok

    def barrier(self):
        import os
        if os.environ.get("KVERBOSE"):
            print("barrier nops", self.nops, dict(self.cnt), {q: d["n"] for q, d in self.dq.items()}, flush=True)
        toks = [(self.sem[e], self.cnt[e], e) for e in self.sem if self.cnt[e] > 0]
        for q, d in self.dq.items():
            n = d["n"]
            for j in range(min(n, self.NDMA)):
                last = ((n - 1 - j) // self.NDMA) * self.NDMA + j
                cntj = (n - 1 - j) // self.NDMA + 1
                toks.append((d["sems"][j], 16 * cntj, q))
        for e in ("pe", "act", "dve", "pool", "sp"):
            waits = self._need(e, toks)
            if waits:
                self.stream[e].append((waits, None, None))
        for b in self.bufs.values():
            b.w = None
            b.rs = []

    def emit(self):
        nc = self.nc
        self.barrier()
        csem = {id(v): k for k, v in self.sem.items()}
        sig = {e: set() for e in self.sem}
        for e in self.stream:
            for waits, fn, inc in self.stream[e]:
                for sem, val in waits:
                    if id(sem) in csem:
                        sig[csem[id(sem)]].add(val)
        rank = {e: {v: i + 1 for i, v in enumerate(sorted(sig[e]))} for e in sig}
        with nc.Block() as block:
            def run(e):
                def body(eng):
                    idx = 0
                    for waits, fn, inc in self.stream[e]:
                        for sem, val in waits:
                            if id(sem) in csem:
                                val = rank[csem[id(sem)]][val]
                            eng.wait_ge(sem, val)
                        if fn is not None:
                            ins = fn(eng)
                            if inc[1] == 16:
                                ins.then_inc(inc[0], 16)
                            else:
                                idx += 1
                                if idx in sig[e]:
                                    ins.then_inc(inc[0], 1)
                return body
            block.sync(run("sp"))
            block.tensor(run("pe"))
            block.scalar(run("act"))
            block.vector(run("dve"))
            block.gpsimd(run("pool"))


def build(NL, taps=(), phases=(2, 31, 32, 33, 41, 42)):
    nc = bass.Bass("TRN2", target_bir_lowering=False)

    def din(name, shape, dt=F32):
        return nc.dram_tensor(name, shape, dt, kind="ExternalInput").ap()

    def dscr(name, shape, dt):
        kind = "ExternalOutput" if name in taps else "Internal"
        return nc.dram_tensor(name, shape, dt, kind=kind).ap()

    x_in = din("x", [S, D])
    w_in = din("w_in", [NL, D, 3584])
    w_out = din("w_out", [NL, D, D])
    w_up = din("w_up", [NL, D, 2 * DFF])
    w_down = din("w_down", [NL, DFF, D])
    ppd = din("pp", [128, NL, NPP])
    pbc = din("pbc", [NL, 2, 128, D])
    cosd = din("cosT", [128, S])
    sind = din("sinT", [128, S])
    cmd = din("cm", [128, 20, 512])
    cstd = din("cst", [128, NCST])
    y = nc.dram_tensor("y", [S, D], F32, kind="ExternalOutput").ap()

    RES = dscr("RES", [S, D], F32)
    QA_T = dscr("QA_T", [2, 128, S], BF16)
    KA_T = dscr("KA_T", [2, 128, S], BF16)
    QC_T = dscr("QC_T", [4, 128, S], BF16)
    KC_T = dscr("KC_T", [4, 128, S], BF16)
    BQ_T = dscr("BQ_T", [2, 2, 128, S], BF16)
    BK_T = dscr("BK_T", [2, 2, 128, S], BF16)
    SG_T = dscr("SG_T", [2, 128, S], BF16)
    TM = dscr("TM", [NT, 128, 1536], BF16)
    EAL = dscr("EAL", [2, 2, 128, 128], F32)
    CC_T = dscr("CC_T", [8, 128, S], BF16)
    HN_T = dscr("HN_T", [8, 128, S + 128], BF16)
    WD = dscr("WD", [22, 128, D], BF16)

    with ExitStack() as es:
        P = Prog(nc, es)
        _ctr = [0]

        from contextlib import contextmanager

        @contextmanager
        def _phase(pid):
            snap = {e: len(P.stream[e]) for e in P.stream}
            cnts = dict(P.cnt)
            dqn = {q: P.dq[q]["n"] for q in P.dq}
            waited = dict(P.waited)
            yield
            if pid not in phases:
                for e in P.stream:
                    del P.stream[e][snap[e]:]
                P.cnt.update(cnts)
                for q in P.dq:
                    P.dq[q]["n"] = dqn[q]
                P.waited.clear()
                P.waited.update(waited)
                for b in P.bufs.values():
                    b.w = None
                    b.rs = []

        def sb(name, shape, dt, st=es):
            _ctr[0] += 1
            return st.enter_context(nc.sbuf_tensor("%s_%d" % (name, _ctr[0]), shape, dt))
        psA = es.enter_context(nc.psum_tensor("psA", [128, 7, 512], F32))
        psT = es.enter_context(nc.psum_tensor("psT", [128, 1024], BF16))
        bank = lambda i: psA[:, i, :]
        psD = psT[:].bitcast(F32)

        def ACT(out, in_, func, reads, writes, **kw):
            return P.op("act", lambda e: e.activation(out=out, in_=in_, func=func, **kw), reads, writes)

        def MM(out, lhsT, rhs, start, stop, reads, writes):
            return P.op("pe", lambda e: e.matmul(out, lhsT=lhsT, rhs=rhs, start=start, stop=stop), reads, writes)

        def TR(out, in_, ident, reads, writes):
            return P.op("pe", lambda e: e.transpose(out=out, in_=in_, identity=ident), reads, writes)

        def TT(eng, out, in0, in1, op, reads, writes):
            return P.op(eng, lambda e: e.tensor_tensor(out=out, in0=in0, in1=in1, op=op), reads, writes)

        def TS(eng, out, in0, s1, s2, op0, op1, reads, writes):
            if s2 is None:
                return P.op(eng, lambda e: e.tensor_scalar(out=out, in0=in0, scalar1=s1, scalar2=None, op0=op0), reads, writes)
            return P.op(eng, lambda e: e.tensor_scalar(out=out, in0=in0, scalar1=s1, scalar2=s2, op0=op0, op1=op1), reads, writes)

        def STT(eng, out, in0, scalar, in1, op0, op1, reads, writes):
            return P.op(eng, lambda e: e.scalar_tensor_tensor(out=out, in0=in0, scalar=scalar, in1=in1, op0=op0, op1=op1), reads, writes)

        def CP(eng, out, in_, reads, writes):
            if eng == "act":
                return P.op("act", lambda e: e.copy(out=out, in_=in_), reads, writes)
            return P.op(eng, lambda e: e.tensor_copy(out=out, in_=in_), reads, writes)

        def tmload(dst, c0, name):
            for g8 in range(8):
                P.dma(dst[:, g8 * 8:(g8 + 1) * 8, :], TM[g8 * 8:(g8 + 1) * 8, :, c0:c0 + 128].rearrange("t p c -> p t c"), writes=[name])

        def rstd_from_ss(rs, ss, n, rname, ssname):
            ACT(rs, ss, AF.Ln, [ssname], [rname], bias=EPS, scale=1.0 / n)
            ACT(rs, rs, AF.Exp, [rname], [rname], scale=-0.5)

        cst = sb("cst", [128, NCST], F32)
        cb = sb("cb", [128, 768], BF16)
        pp = sb("pp", [128, NL, NPP], F32)
        lbs = sb("lbs", [128, 2, 2, 4], F32)
        oml = sb("oml", [128, 2, 2, 4], F32)
        lbt = sb("lbt", [128, 4, 4], F32)
        lbsum = sb("lbsum", [128, 4], F32)
        lam = sb("lam", [128, NL, 4], F32)
        lamt = sb("lamt", [128, 128], F32)
        P.dma(cst[:], cstd[:, :], writes=["cst"])
        P.dma(pp[:], ppd[:, :, :], writes=["pp"])
        CP("dve", cb[:], cst[:, 0:768], ["cst"], ["cb"])
        ident = cb[:, 0:128]
        Rm = cb[:, 128:256]
        ones = cb[:, 256:384]
        bd64 = cb[:, 384:512]
        maskF = cb[:, 512:640]
        maskB = cb[:, 640:768]
        scanm = cst[:, 768:1280]
        lg = pp[:, 0, 192:208].rearrange("p (a l) -> p a l", l=4)
        ACT(lbt[:], lg, AF.Exp, ["pp"], ["lbt"])
        P.op("dve", lambda e: e.tensor_reduce(out=lbsum[:], in_=lbt[:], axis=AX.X, op=ALU.add), ["lbt"], ["lbsum"])
        P.op("dve", lambda e: e.reciprocal(out=lbsum[:], in_=lbsum[:]), ["lbsum"], ["lbsum"])
        TT("dve", lbt[:], lbt[:], lbsum[:].unsqueeze(2).broadcast_to([128, 4, 4]), ALU.mult, ["lbt", "lbsum"], ["lbt"])
        lbs3 = lbs[:].rearrange("p a b l -> p (a b) l")
        oml3 = oml[:].rearrange("p a b l -> p (a b) l")
        P.op("dve", lambda e: e.memset(lbs[:], 0.0), [], ["lbs"])
        for l in range(1, 4):
            TT("dve", lbs3[:, :, l:l + 1], lbs3[:, :, l - 1:l], lbt[:, :, l:l + 1], ALU.add, ["lbs", "lbt"], ["lbs"])
        TS("dve", oml3, lbs3, -1.0, 1.0, ALU.mult, ALU.add, ["lbs"], ["oml"])
        for l in range(NL):
            dl = pp[:, l, 212:468]
            TT("dve", lamt[:, 0:64], dl[:, 0:64], dl[:, 64:128], ALU.mult, ["pp"], ["lamt"])
            TT("dve", lamt[:, 64:128], dl[:, 128:192], dl[:, 192:256], ALU.mult, ["pp"], ["lamt"])
            P.op("dve", lambda e, l=l: e.tensor_reduce(out=lam[:, l, 2:4], in_=lamt[:].rearrange("p (a b) -> p a b", a=2), axis=AX.X, op=ALU.add), ["lamt"], ["lam"])
            ACT(lam[:, l, 2:4], lam[:, l, 2:4], AF.Exp, ["lam"], ["lam"])
            TT("dve", lam[:, l, 0:1], lam[:, l, 3:4], lam[:, l, 2:3], ALU.subtract, ["lam"], ["lam"])
            TT("dve", lam[:, l, 0:1], lam[:, l, 0:1], pp[:, l, 210:211], ALU.subtract, ["lam", "pp"], ["lam"])
            TT("dve", lam[:, l, 1:2], pp[:, l, 209:210], pp[:, l, 211:212], ALU.mult, ["pp"], ["lam"])

        for l in range(NL):
            xsrc = x_in if l == 0 else RES
            xdst = y if l == NL - 1 else RES
            with ExitStack() as ph, _phase(2):
                win = sb("win", [128, 8, 3584], BF16, ph)
                wst = [sb("wst%d" % i, [128, 8, 256], F32, ph) for i in range(2)]
                xt = sb("xt", [128, D], F32, ph)
                junk = sb("junk", [128, D], BF16, ph)
                xs = sb("xs", [128, D], BF16, ph)
                xnT = sb("xnT", [128, 8, 512], BF16, ph)
                cosb = sb("cosb", [128, 512], F32, ph)
                sinb = sb("sinb", [128, 512], F32, ph)
                ofm = sb("ofm", [128, 22, 512], BF16, ph)
                otm = sb("otm", [128, 4, 1536], BF16, ph)
                st1 = sb("st1", [128, 2], F32, ph)
                qraw = sb("qraw", [128, 512], BF16, ph)
                t1 = sb("t1", [128, 512], F32, ph)
                t2 = sb("t2", [128, 512], F32, ph)
                qf = sb("qf", [128, 512], F32, ph)
                ff = sb("ff", [128, 512], F32, ph)
                gg = sb("gg", [128, 512], F32, ph)
                kk = sb("kk", [128, 512], F32, ph)
                pref = sb("pref", [128, 512], F32, ph)
                dd = sb("dd", [128, 512], F32, ph)
                aa = sb("aa", [128, 512], F32, ph)
                ee = sb("ee", [128, 512], F32, ph)
                khT = sb("khT", [128, 512], BF16, ph)
                ealb = sb("ealb", [128, 2, 2, NB, 8], F32, ph)

                for c in range(14):
                    w = wst[c % 2]
                    P.dma(w[:], w_in[l][:, c * 256:(c + 1) * 256].rearrange("(k p) n -> p k n", p=128), writes=["wst%d" % (c % 2)])
                    TT("pool", win[:, :, c * 256:(c + 1) * 256], w[:], pp[:, l, 0:8].unsqueeze(2).broadcast_to([128, 8, 256]),
                       ALU.mult, ["wst%d" % (c % 2), "pp"], ["win"])

                for tb in range(NB):
                    t0 = tb * 512
                    P.dma(cosb[:], cosd[:, t0:t0 + 512], writes=["cosb"])
                    P.dma(sinb[:], sind[:, t0:t0 + 512], writes=["sinb"])
                    for ti in range(4):
                        r0 = t0 + ti * 128
                        P.dma(xt[:], xsrc[r0:r0 + 128, :], writes=["xt"])
                        ACT(junk[:], xt[:], AF.Square, ["xt"], ["junk", "st1"], accum_out=st1[:, 0:1])
                        rstd_from_ss(st1[:, 1:2], st1[:, 0:1], D, "st1", "st1")
                        TS("dve", xs[:], xt[:], st1[:, 1:2], None, ALU.mult, None, ["xt", "st1"], ["xs"])
                        for k in range(8):
                            TR(psT[:, k * 128:(k + 1) * 128], xs[:, k * 128:(k + 1) * 128], ident, ["xs", "cb"], ["bT"])
                        CP("dve", xnT[:, :, ti * 128:(ti + 1) * 128], psT[:].rearrange("p (k t) -> p k t", k=8), ["bT"], ["xnT"])

                    def proj(bi, c0):
                        for k in range(8):
                            MM(bank(bi), win[:, k, c0:c0 + 128], xnT[:, k, :], k == 0, k == 7, ["win", "xnT"], ["b%d" % bi])

                    ropes = [(0, 0, QA_T[0]), (1, 128, QA_T[1]), (2, 256, KA_T[0]), (3, 384, KA_T[1])]
                    for h in range(4):
                        ropes.append((4 + h, 2048 + h * 128, QC_T[h]))
                        ropes.append((8 + h, 2560 + h * 128, KC_T[h]))
                    for ri, (oi, c0, dst) in enumerate(ropes):
                        bi = ri % 2
                        proj(bi, c0)
                        CP("act", qraw[:], bank(bi), ["b%d" % bi], ["qraw"])
                        MM(bank(2 + bi), Rm, qraw[:], True, True, ["qraw", "cb"], ["b%d" % (2 + bi)])
                        TT("dve", t1[:], bank(bi), cosb[:], ALU.mult, ["b%d" % bi, "cosb"], ["t1"])
                        TT("dve", t2[:], bank(2 + bi), sinb[:], ALU.mult, ["b%d" % (2 + bi), "sinb"], ["t2"])
                        TT("dve", ofm[:, oi, :], t1[:], t2[:], ALU.add, ["t1", "t2"], ["ofm%d" % oi])
                        P.dma(dst[:, t0:t0 + 512], ofm[:, oi, :], reads=["ofm%d" % oi])

                    for ti in range(4):
                        for gi, (c0, n, o0) in enumerate([(512, 256, 0), (1536, 256, 256), (3072, 512, 512)]):
                            bi = 4 + (ti * 3 + gi) % 2
                            for k in range(8):
                                MM(bank(bi)[:, 0:n], xnT[:, k, ti * 128:(ti + 1) * 128], win[:, k, c0:c0 + n], k == 0, k == 7,
                                   ["xnT", "win"], ["b%d" % bi])
                            CP("act", otm[:, ti, o0:o0 + n], bank(bi)[:, 0:n], ["b%d" % bi], ["otmv%d" % ti])

                    for p in range(2):
                        proj(0, 1792 + p * 128)
                        ACT(ofm[:, 20 + p, :], bank(0), AF.Silu, ["b0"], ["ofm%d" % (20 + p)])
                        P.dma(SG_T[p][:, t0:t0 + 512], ofm[:, 20 + p, :], reads=["ofm%d" % (20 + p)])
                        proj(1, 768 + p * 128)
                        CP("act", qf[:], bank(1), ["b1"], ["qf"])
                        for di in range(2):
                            proj(2, (1024 if di == 0 else 1280) + p * 128)
                            ACT(ff[:], bank(2), AF.Sigmoid, ["b2"], ["ff"])
                            TS("dve", ff[:], ff[:], oml[:, di, p, l:l + 1], lbs[:, di, p, l:l + 1], ALU.mult, ALU.add, ["ff", "oml", "lbs"], ["ff"])
                            ACT(gg[:], ff[:], AF.Ln, ["ff"], ["gg"])
                            TS("dve", kk[:], ff[:], -1.0, 1.0, ALU.mult, ALU.add, ["ff"], ["kk"])
                            P.op("dve", lambda e: e.tensor_tensor_scan(out=pref[:], data0=scanm, data1=gg[:], initial=0.0, op0=ALU.mult, op1=ALU.add),
                                 ["gg", "cst"], ["pref"])
                            pref3 = pref[:].rearrange("p (c t) -> p c t", t=64)
                            tot = pref3[:, :, 63:64]
                            TT("dve", dd[:].rearrange("p (c t) -> p c t", t=64), tot.broadcast_to([128, 8, 64]), pref3, ALU.subtract, ["pref"], ["dd"])
                            if di == 0:
                                a_ap, a_nm = pref, "pref"
                                dk_ap, dk_nm = dd, "dd"
                            else:
                                TT("dve", aa[:], dd[:], gg[:], ALU.add, ["dd", "gg"], ["aa"])
                                TT("dve", dd[:], pref[:], gg[:], ALU.subtract, ["pref", "gg"], ["dd"])
                                a_ap, a_nm = aa, "aa"
                                dk_ap, dk_nm = dd, "dd"
                            oq = 12 + p * 4 + di * 2
                            ACT(ee[:], a_ap[:], AF.Exp, [a_nm], ["ee"])
                            TT("dve", ofm[:, oq, :], qf[:], ee[:], ALU.mult, ["qf", "ee"], ["ofm%d" % oq])
                            P.dma(BQ_T[p, di][:, t0:t0 + 512], ofm[:, oq, :], reads=["ofm%d" % oq])
                            ACT(ee[:], a_ap[:], AF.Exp, [a_nm], ["ee"], scale=-1.0)
                            TT("dve", ofm[:, oq + 1, :], kk[:], ee[:], ALU.mult, ["kk", "ee"], ["ofm%d" % (oq + 1)])
                            P.dma(BK_T[p, di][:, t0:t0 + 512], ofm[:, oq + 1, :], reads=["ofm%d" % (oq + 1)])
                            ACT(ee[:], dk_ap[:], AF.Exp, [dk_nm], ["ee"])
                            TT("dve", khT[:], kk[:], ee[:], ALU.mult, ["kk", "ee"], ["khT"])
                            for ti in range(4):
                                TR(psT[:, ti * 128:(ti + 1) * 128], khT[:, ti * 128:(ti + 1) * 128], ident, ["khT", "cb"], ["bT"])
                            o0 = 1024 + di * 256 + p * 128
                            CP("dve", otm[:, :, o0:o0 + 128], psT[:, 0:512].rearrange("p (t c) -> p t c", t=4), ["bT"], ["otmk"])
                            ACT(ealb[:, p, di, tb, :], tot.rearrange("p c o -> p (c o)"), AF.Exp, ["pref"], ["ealb"])
                    for ti in range(4):
                        P.dma(TM[tb * 4 + ti], otm[:, ti, :], reads=["otmv%d" % ti, "otmk"])
                for p in range(2):
                    for di in range(2):
                        P.dma(EAL[p, di], ealb[:, p, di].rearrange("p b c -> p (b c)"), reads=["ealb"])
                P.barrier()

            with ExitStack() as ph, _phase(31):
                cmf = sb("cmf", [128, 4, 512], F32, ph)
                cmb = sb("cmb", [128, 20, 512], BF16, ph)
                qT = sb("qT", [128, S], BF16, ph)
                kT = sb("kT", [128, S], BF16, ph)
                vv = sb("vv", [128, NT, 128], BF16, ph)
                pT = [sb("pT%d" % i, [128, 512], BF16, ph) for i in range(3)]
                pm = [sb("pm%d" % i, [128, 512], BF16, ph) for i in range(3)]
                rden = sb("rden", [128, 512], F32, ph)
                osb = [sb("osb%d" % i, [128, 512], BF16, ph) for i in range(2)]
                for c in range(5):
                    P.dma(cmf[:], cmd[:, c * 4:(c + 1) * 4, :], writes=["cmf"])
                    CP("dve", cmb[:, c * 4:(c + 1) * 4, :], cmf[:], ["cmf"], ["cmb"])
                for p in range(2):
                    P.dma(qT[:], QA_T[p], writes=["qT"])
                    P.dma(kT[:], KA_T[p], writes=["kT"])
                    tmload(vv, p * 128, "vv")
                    units = []
                    for qb in range(NB):
                        for h in range(2):
                            kcs = [kc for kc in range(4 * qb - 8, 4 * qb + 12) if 0 <= kc < NT]
                            for j, kc in enumerate(kcs):
                                units.append((qb, h, kc, j == 0, j == len(kcs) - 1))

                    def smm(idx):
                        qb, h, kc, first, last = units[idx]
                        hr = slice(h * 64, (h + 1) * 64)
                        bi = idx % 3
                        MM(bank(bi), kT[hr, kc * 128:(kc + 1) * 128], qT[hr, qb * 512:(qb + 1) * 512], True, True, ["kT", "qT"], ["b%d" % bi])

                    smm(0)
                    smm(1)
                    for idx, (qb, h, kc, first, last) in enumerate(units):
                        if idx + 2 < len(units):
                            smm(idx + 2)
                        hr = slice(h * 64, (h + 1) * 64)
                        bi = idx % 3
                        rel = kc - 4 * qb + 8
                        ob = osb[qb % 2]
                        obn = "osb%d" % (qb % 2)
                        ACT(pT[bi][:], bank(bi), AF.Exp, ["b%d" % bi], ["pT%d" % bi], scale=0.125)
                        TT("dve", pm[bi][:], pT[bi][:], cmb[:, rel, :], ALU.mult, ["pT%d" % bi, "cmb"], ["pm%d" % bi])
                        MM(bank(3), vv[:, kc, :], pm[bi][:], first, last, ["vv", "pm%d" % bi], ["b3"])
                        MM(bank(4), ones, pm[bi][:], first, last, ["cb", "pm%d" % bi], ["b4"])
                        if last:
                            P.op("dve", lambda e, hr=hr: e.reciprocal(out=rden[hr, :], in_=bank(4)[hr, :]), ["b4"], ["rden"])
                            TT("dve", ob[hr, :], bank(3)[hr, :], rden[hr, :], ALU.mult, ["b3", "rden"], [obn])
                            if h == 1:
                                P.dma(CC_T[p][:, qb * 512:(qb + 1) * 512], ob[:], reads=[obn])
                P.barrier()

            with ExitStack() as ph, _phase(32):
                qT = sb("hqT", [128, S], BF16, ph)
                kT = sb("hkT", [128, S], BF16, ph)
                kh = sb("hkh", [128, NT, 128], BF16, ph)
                vv = sb("hvv", [128, NT, 128], BF16, ph)
                obuf = sb("obuf", [128, S], F32, ph)
                eal = sb("eal", [128, 128], F32, ph)
                Sf = sb("Sf", [128, 128], F32, ph)
                Sb = sb("Sb", [128, 128], BF16, ph)
                sT = [sb("sT%d" % i, [128, 2, 128], BF16, ph) for i in range(2)]
                mD = sb("mD", [128, 2, 2, 128], BF16, ph)
                sq = sb("sq", [128, 512], BF16, ph)
                rs = sb("rs", [128, 512], F32, ph)
                tq = sb("tq", [128, 512], F32, ph)
                sgb = [sb("sgb%d" % i, [128, 512], BF16, ph) for i in range(2)]
                ocb = [sb("ocb%d" % i, [128, 512], BF16, ph) for i in range(2)]
                P.op("dve", lambda e: e.memset(mD[:], 0.0), [], ["mD"])
                for di, mk in enumerate((maskF, maskB)):
                    for h in range(2):
                        CP("dve", mD[0:64, di, h, 0:64], mk[0:64, 0:64], ["cb", "mD"], ["mD"])
                        CP("dve", mD[64:128, di, h, 64:128], mk[64:128, 64:128], ["cb", "mD"], ["mD"])
                for p in range(2):
                    tmload(vv, 256 + p * 128, "hvv")
                    for di in range(2):
                        P.dma(qT[:], BQ_T[p, di], writes=["hqT"])
                        P.dma(kT[:], BK_T[p, di], writes=["hkT"])
                        o0 = 1024 + di * 256 + p * 128
                        tmload(kh, o0, "hkh")
                        P.dma(eal[:], EAL[p, di], writes=["eal"])
                        P.op("dve", lambda e: e.memset(Sf[:], 0.0), [], ["Sf"])
                        P.op("dve", lambda e: e.memset(Sb[:], 0.0), [], ["Sb"])
                        tiles = range(NT) if di == 0 else range(NT - 1, -1, -1)
                        for n, ti in enumerate(tiles):
                            tc0 = ti * 128
                            sb_i = n % 2
                            for h in range(2):
                                hr = slice(h * 64, (h + 1) * 64)
                                sbk, sbn = (bank(sb_i), "b%d" % sb_i) if h == 0 else (bank(6), "b6")
                                MM(sbk[:, 0:128], kT[hr, tc0:tc0 + 128], qT[hr, tc0:tc0 + 128], True, True, ["hkT", "hqT"], [sbn])
                                TT("dve", sT[sb_i][:, h, :], sbk[:, 0:128], mD[:, di, h, :], ALU.mult, [sbn, "mD"], ["sT%d" % sb_i])
                            ob_i = 2 + n % 2
                            for cc in ((0, 1) if di == 0 else (1, 0)):
                                c = ti * 2 + cc
                                cs = slice(cc * 64, (cc + 1) * 64)
                                for h in range(2):
                                    hr = slice(h * 64, (h + 1) * 64)
                                    oo = bank(ob_i)[:, h * 128 + cc * 64:h * 128 + (cc + 1) * 64]
                                    MM(oo, vv[:, ti, :], sT[sb_i][:, h, cs], True, False, ["hvv", "sT%d" % sb_i], ["b%d" % ob_i])
                                    MM(oo, Sb[hr, :], qT[hr, tc0 + cc * 64:tc0 + (cc + 1) * 64], False, True, ["Sb", "hqT"], ["b%d" % ob_i])
                                MM(bank(4)[:, 0:128], kh[cs, ti, :], vv[cs, ti, :], True, True, ["hkh", "hvv"], ["b4"])
                                STT("dve", Sf[:], Sf[:], eal[:, c:c + 1], bank(4)[:, 0:128], ALU.mult, ALU.add, ["Sf", "eal", "b4"], ["Sf"])
                                CP("act", Sb[:], Sf[:], ["Sf"], ["Sb"])
                            for h in range(2):
                                hr = slice(h * 64, (h + 1) * 64)
                                src = bank(ob_i)[hr, h * 128:(h + 1) * 128]
                                if di == 0:
                                    CP("act", obuf[hr, tc0:tc0 + 128], src, ["b%d" % ob_i], ["obuf%d" % ti])
                                else:
                                    TT("dve", obuf[hr, tc0:tc0 + 128], obuf[hr, tc0:tc0 + 128], src, ALU.add, ["b%d" % ob_i, "obuf%d" % ti], ["obuf%d" % ti])
                    for tb in range(NB):
                        t0 = tb * 512
                        sl = slice(t0, t0 + 512)
                        rn = ["obuf%d" % (tb * 4 + i) for i in range(4)]
                        i2 = tb % 2
                        P.dma(sgb[i2][:], SG_T[p][:, sl], writes=["sgb%d" % i2])
                        ACT(sq[:], obuf[:, sl], AF.Square, rn, ["sq"])
                        MM(bank(5), bd64, sq[:], True, True, ["cb", "sq"], ["b5"])
                        rstd_from_ss(rs[:], bank(5), 64, "rs", "b5")
                        TT("dve", tq[:], obuf[:, sl], rs[:], ALU.mult, rn + ["rs"], ["tq"])
                        STT("dve", ocb[i2][:], tq[:], pp[:, l, 208:209], sgb[i2][:], ALU.mult, ALU.mult, ["tq", "pp", "sgb%d" % i2], ["ocb%d" % i2])
                        P.dma(CC_T[2 + p][:, sl], ocb[i2][:], reads=["ocb%d" % i2])
                P.barrier()

            with ExitStack() as ph, _phase(33):
                qT = sb("cqT", [128, S], BF16, ph)
                kT = sb("ckT", [128, S], BF16, ph)
                vv = sb("cvv", [128, NT, 128], BF16, ph)
                pT = [sb("cpT%d" % i, [128, 2, 512], BF16, ph) for i in range(2)]
                r1 = sb("r1", [128, 512], F32, ph)
                r2 = sb("r2", [128, 512], F32, ph)
                u1 = sb("u1", [128, 512], F32, ph)
                u2 = sb("u2", [128, 512], F32, ph)
                sq = sb("csq", [128, 512], BF16, ph)
                rs = sb("crs", [128, 512], F32, ph)
                ocb = [sb("cocb%d" % i, [128, 512], BF16, ph) for i in range(2)]
                acc = [sb("acc%d" % i, [128, 2, 512], F32, ph) for i in range(2)]
                onesf = cst[:, 256:384]
                for h in range(4):
                    P.dma(qT[:], QC_T[h], writes=["cqT"])
                    P.dma(kT[:], KC_T[h], writes=["ckT"])
                    tmload(vv, 512 + h * 128, "cvv")
                    units = [(qb, kc) for qb in range(NB) for kc in range(NT)]

                    def qk(idx):
                        qb, kc = units[idx]
                        i2 = idx % 2
                        for m in range(2):
                            mr = slice(m * 64, (m + 1) * 64)
                            MM(bank(2 * i2 + m), kT[mr, kc * 128:(kc + 1) * 128], qT[mr, qb * 512:(qb + 1) * 512], True, True, ["ckT", "cqT"], ["s%d" % i2])

                    qk(0)
                    for idx, (qb, kc) in enumerate(units):
                        if idx + 1 < len(units):
                            qk(idx + 1)
                        i2 = idx % 2
                        qs = slice(qb * 512, (qb + 1) * 512)
                        ACT(pT[i2][:], psA[:, 2 * i2:2 * i2 + 2, :], AF.Exp, ["s%d" % i2], ["cpT%d" % i2], scale=0.125)
                        for m in range(2):
                            MM(bank(4 + m), vv[:, kc, :], pT[i2][:, m, :], kc == 0, kc == NT - 1, ["cvv", "cpT%d" % i2], ["o%d" % m])
                        a2 = qb % 2
                        if kc == 0:
                            CP("dve", acc[a2][:, 0, :], pT[i2][:, 0, :], ["cpT%d" % i2], ["acc%d" % a2])
                        else:
                            TT("dve", acc[a2][:, 0, :], acc[a2][:, 0, :], pT[i2][:, 0, :], ALU.add, ["acc%d" % a2, "cpT%d" % i2], ["acc%d" % a2])
                        MM(psD, ones, pT[i2][:, 1, :], kc == 0, kc == NT - 1, ["cb", "cpT%d" % i2], ["d1"])
                        if kc == NT - 1:
                            MM(bank(6), onesf, acc[a2][:, 0, :], True, True, ["cst", "acc%d" % a2], ["d0"])
                            P.op("dve", lambda e: e.reciprocal(out=r1[:], in_=bank(6)), ["d0"], ["r1"])
                            P.op("dve", lambda e: e.reciprocal(out=r2[:], in_=psD), ["d1"], ["r2"])
                            TT("dve", u1[:], bank(4), r1[:], ALU.mult, ["o0", "r1"], ["u1"])
                            TT("dve", u2[:], bank(5), r2[:], ALU.mult, ["o1", "r2"], ["u2"])
                            STT("dve", u1[:], u2[:], lam[:, l, 0:1], u1[:], ALU.mult, ALU.add, ["u1", "u2", "lam"], ["u1"])
                            ACT(sq[:], u1[:], AF.Square, ["u1"], ["csq"])
                            MM(bank(6), ones, sq[:], True, True, ["cb", "csq"], ["d0"])
                            rstd_from_ss(rs[:], bank(6), 128, "crs", "d0")
                            o2 = qb % 2
                            TT("dve", u2[:], u1[:], rs[:], ALU.mult, ["u1", "crs"], ["u2"])
                            TS("dve", ocb[o2][:], u2[:], lam[:, l, 1:2], None, ALU.mult, None, ["u2", "lam"], ["cocb%d" % o2])
                            P.dma(CC_T[4 + h][:, qs], ocb[o2][:], reads=["cocb%d" % o2])
                P.barrier()

            with ExitStack() as ph, _phase(41):
                wo = sb("wo", [128, 8, D], BF16, ph)
                wst = [sb("wst%d" % i, [128, 8, 256], F32, ph) for i in range(2)]
                ccb = [sb("ccb%d" % i, [128, 8, 512], BF16, ph) for i in range(2)]
                xt = [sb("xt%d" % i, [128, D], F32, ph) for i in range(2)]
                gpo = sb("gpo", [128, D], F32, ph)
                tm = sb("tm", [128, D], F32, ph)
                hh = [sb("hh%d" % i, [128, D], F32, ph) for i in range(2)]
                junk = sb("junk", [128, D], BF16, ph)
                hs = sb("hs", [128, D], BF16, ph)
                hT = [sb("hT%d" % i, [128, 8, 512], BF16, ph) for i in range(2)]
                st1 = sb("st1", [128, 4], F32, ph)
                zz = sb("zz", [128, 8, 64], BF16, ph)
                P.op("dve", lambda e: e.memset(zz[:], 0.0), [], ["zz"])
                P.dma(HN_T[:, :, 0:64].rearrange("k p o -> p k o"), zz[:], reads=["zz"])
                P.dma(HN_T[:, :, S + 64:S + 128].rearrange("k p o -> p k o"), zz[:], reads=["zz"])
                P.dma(gpo[:], pbc[l, 0], writes=["gpo"])
                for c in range(4):
                    w = wst[c % 2]
                    P.dma(w[:], w_out[l][:, c * 256:(c + 1) * 256].rearrange("(k p) n -> p k n", p=128), writes=["wst%d" % (c % 2)])
                    CP("pool", wo[:, :, c * 256:(c + 1) * 256], w[:], ["wst%d" % (c % 2)], ["wo"])
                for tb in range(NB):
                    t0 = tb * 512
                    b2 = tb % 2
                    P.dma(ccb[b2][:], CC_T[:, :, t0:t0 + 512].rearrange("k p t -> p k t"), writes=["ccb%d" % b2])
                    for ti in range(4):
                        r0 = t0 + ti * 128
                        n = tb * 4 + ti
                        i2 = n % 2
                        P.dma(xt[i2][:], xsrc[r0:r0 + 128, :], writes=["xt%d" % i2])
                        pb = 2 * i2
                        for half in range(2):
                            for k in range(8):
                                MM(bank(pb + half), ccb[b2][:, k, ti * 128:(ti + 1) * 128], wo[:, k, half * 512:(half + 1) * 512], k == 0, k == 7,
                                   ["ccb%d" % b2, "wo"], ["m%d" % i2])
                        mix = psA[:, pb:pb + 2, :].rearrange("p a b -> p (a b)")
                        ACT(junk[:], mix, AF.Square, ["m%d" % i2], ["junk", "st1"], accum_out=st1[:, 0:1])
                        rstd_from_ss(st1[:, 1:2], st1[:, 0:1], D, "st1", "st1")
                        STT("dve", tm[:], mix, st1[:, 1:2], gpo[:], ALU.mult, ALU.mult, ["m%d" % i2, "st1", "gpo"], ["tm"])
                        TT("dve", hh[i2][:], tm[:], xt[i2][:], ALU.add, ["tm", "xt%d" % i2], ["hh%d" % i2])
                        P.dma(RES[r0:r0 + 128, :], hh[i2][:], reads=["hh%d" % i2])
                        ACT(junk[:], hh[i2][:], AF.Square, ["hh%d" % i2], ["junk", "st1"], accum_out=st1[:, 2:3])
                        rstd_from_ss(st1[:, 3:4], st1[:, 2:3], D, "st1", "st1")
                        TS("dve", hs[:], hh[i2][:], st1[:, 3:4], None, ALU.mult, None, ["hh%d" % i2, "st1"], ["hs"])
                        for k in range(8):
                            TR(psT[:, k * 128:(k + 1) * 128], hs[:, k * 128:(k + 1) * 128], ident, ["hs", "cb"], ["bT"])
                        CP("dve", hT[b2][:, :, ti * 128:(ti + 1) * 128], psT[:].rearrange("p (k t) -> p k t", k=8), ["bT"], ["hT%d" % b2])
                    P.dma(HN_T[:, :, 64 + t0:64 + t0 + 512].rearrange("k p t -> p k t"), hT[b2][:], reads=["hT%d" % b2])
                P.barrier()

            with ExitStack() as ph, _phase(42):
                wup = sb("wup", [128, 8, 2 * DFF], BF16, ph)
                wst = [sb("wst%d" % i, [128, 8, 256], F32, ph) for i in range(2)]
                wdb = [sb("wdb%d" % i, [128, D], BF16, ph) for i in range(4)]
                hnT = [sb("hnT%d" % i, [128, 8, 514], BF16, ph) for i in range(2)]
                U = [sb("U%d" % i, [128, 514], F32, ph) for i in range(2)]
                cv = [sb("cv%d" % i, [128, 512], F32, ph) for i in range(2)]
                sgl = sb("sgl", [128, 512], F32, ph)
                actT = sb("actT", [128, 22, 512], BF16, ph)
                hh = [sb("hh%d" % i, [128, D], F32, ph) for i in range(2)]
                gpo = sb("gpo", [128, D], F32, ph)
                oo = [sb("oo%d" % i, [128, D], F32, ph) for i in range(2)]
                junk = sb("junk", [128, D], BF16, ph)
                st1 = sb("st1", [128, 2], F32, ph)
                P.dma(gpo[:], pbc[l, 1], writes=["gpo"])
                for j in range(22):
                    w = wst[j % 2]
                    wv = w[:].rearrange("p k n -> p (k n)")[:, 0:D]
                    P.dma(wv, w_down[l][j * 128:(j + 1) * 128, :], writes=["wst%d" % (j % 2)])
                    CP("pool", wdb[j % 4][:], wv, ["wst%d" % (j % 2)], ["wdb%d" % (j % 4)])
                    P.dma(WD[j], wdb[j % 4][:], reads=["wdb%d" % (j % 4)], writes=["WD"])
                for c in range(22):
                    w = wst[c % 2]
                    P.dma(w[:], w_up[l][:, c * 256:(c + 1) * 256].rearrange("(k p) n -> p k n", p=128), writes=["wst%d" % (c % 2)])
                    TT("pool", wup[:, :, c * 256:(c + 1) * 256], w[:], pp[:, l, 8:16].unsqueeze(2).broadcast_to([128, 8, 256]),
                       ALU.mult, ["wst%d" % (c % 2), "pp"], ["wup"])
                wdi = 0
                for tb in range(NB):
                    t0 = tb * 512
                    b2 = tb % 2
                    hn = hnT[b2]
                    hnn = "hnT%d" % b2
                    P.dma(hn[:], HN_T[:, :, 63 + t0:63 + t0 + 514].rearrange("k p t -> p k t"), writes=[hnn])
                    halo = hn[:, :, 0:514:513]
                    for j in range(22):
                        for gv in range(2):
                            cidx = j + 22 * gv
                            c0 = cidx * 128
                            bi = gv
                            for k in range(8):
                                MM(bank(bi), wup[:, k, c0:c0 + 128], hn[:, k, 1:513], k == 0, k == 7, ["wup", hnn], ["b%d" % bi])
                            for k in range(8):
                                MM(bank(6)[:, gv * 2:gv * 2 + 2], wup[:, k, c0:c0 + 128], hn[:, k, 0:514:513], k == 0, k == 7, ["wup", hnn], ["h6"])
                            Ub = U[gv]
                            un = "U%d" % gv
                            CP("act", Ub[:, 1:513], bank(bi), ["b%d" % bi], [un])
                            CP("act", Ub[:, 0:514:513], bank(6)[:, gv * 2:gv * 2 + 2], ["h6"], [un])
                            cw = lambda r: pp[:, l, 16 + r * 44 + cidx:16 + r * 44 + cidx + 1]
                            cbias = pp[:, l, 148 + cidx:148 + cidx + 1]
                            TS("dve", cv[gv][:], Ub[:, 1:513], cw(1), cbias, ALU.mult, ALU.add, [un, "pp"], ["cv%d" % gv])
                            STT("dve", cv[gv][:], Ub[:, 0:512], cw(0), cv[gv][:], ALU.mult, ALU.add, [un, "pp", "cv%d" % gv], ["cv%d" % gv])
                            STT("dve", cv[gv][:], Ub[:, 2:514], cw(2), cv[gv][:], ALU.mult, ALU.add, [un, "pp", "cv%d" % gv], ["cv%d" % gv])
                        ACT(sgl[:], cv[0][:], AF.Silu, ["cv0"], ["sgl"])
                        TT("dve", actT[:, j, :], sgl[:], cv[1][:], ALU.mult, ["sgl", "cv1"], ["actT"])
                    for ps_ in range(2):
                        for j in range(22):
                            wb = wdb[wdi % 4]
                            wbn = "wdb%d" % (wdi % 4)
                            wdi += 1
                            P.dma(wb[:], WD[j], reads=["WD"], writes=[wbn])
                            for tt_ in range(2):
                                ti = ps_ * 2 + tt_
                                for half in range(2):
                                    MM(bank(2 + tt_ * 2 + half), actT[:, j, ti * 128:(ti + 1) * 128], wb[:, half * 512:(half + 1) * 512],
                                       j == 0, j == 21, ["actT", wbn], ["f%d" % tt_])
                        for tt_ in range(2):
                            ti = ps_ * 2 + tt_
                            r0 = t0 + ti * 128
                            i2 = tt_
                            P.dma(hh[i2][:], RES[r0:r0 + 128, :], writes=["hh%d" % i2])
                            fo = psA[:, 2 + tt_ * 2:4 + tt_ * 2, :].rearrange("p a b -> p (a b)")
                            ACT(junk[:], fo, AF.Square, ["f%d" % tt_], ["junk", "st1"], accum_out=st1[:, 0:1])
                            rstd_from_ss(st1[:, 1:2], st1[:, 0:1], D, "st1", "st1")
                            STT("dve", oo[i2][:], fo, st1[:, 1:2], gpo[:], ALU.mult, ALU.mult, ["f%d" % tt_, "st1", "gpo"], ["oo%d" % i2])
                            TT("dve", oo[i2][:], oo[i2][:], hh[i2][:], ALU.add, ["oo%d" % i2, "hh%d" % i2], ["oo%d" % i2])
                            P.dma(xdst[r0:r0 + 128, :], oo[i2][:], reads=["oo%d" % i2])
                P.barrier()
        P.emit()
    return nc


def _consts():
    pos = np.arange(S, dtype=np.float32)
    inv = (np.float32(500000.0) ** (-np.arange(0, 16, 2, dtype=np.float32) / np.float32(16))).astype(np.float32)
    ang = pos[None, :] * inv[:, None]
    cosT = np.ones((128, S), np.float32)
    sinT = np.zeros((128, S), np.float32)
    for p in range(128):
        j = p % 64
        if j < 8:
            cosT[p] = np.cos(ang[j]); sinT[p] = -np.sin(ang[j])
        elif j < 16:
            cosT[p] = np.cos(ang[j - 8]); sinT[p] = np.sin(ang[j - 8])
    cm = np.zeros((128, 20, 512), np.float32)
    i = np.arange(128)[:, None]
    jq = np.arange(512)[None, :]
    for rel in range(20):
        d = (rel - 8) * 128 + i - jq
        ad = np.abs(d)
        cm[:, rel, :] = (ad <= 64).astype(np.float32) + ((d % 4 == 0) & (ad <= 256)) + ((d % 16 == 0) & (ad <= 1024))
    cst = np.zeros((128, NCST), np.float32)
    cst[:, 0:128] = np.eye(128)
    Rm = np.zeros((128, 128), np.float32)
    for m in range(128):
        j = m % 64
        if j < 8:
            Rm[m + 8, m] = 1.0
        elif j < 16:
            Rm[m - 8, m] = 1.0
    cst[:, 128:256] = Rm
    cst[:, 256:384] = 1.0
    bd = np.zeros((128, 128), np.float32)
    bd[0:64, 0:64] = 1.0
    bd[64:128, 64:128] = 1.0
    cst[:, 384:512] = bd
    s_ = np.arange(128)[:, None] % 64
    t_ = np.arange(128)[None, :] % 64
    cst[:, 512:640] = (s_ <= t_)
    cst[:, 640:768] = (s_ >= t_)
    m = np.ones(512, np.float32)
    m[0::64] = 0.0
    cst[:, 768:1280] = m[None, :]
    return cosT, sinT, cm, cst


def _pack_params(inp, layers):
    NL = len(layers)
    pp = np.zeros((128, NL, NPP), np.float32)
    pbc = np.zeros((NL, 2, 128, D), np.float32)
    lb = inp["lb_logits"]
    lbl = np.transpose(lb.reshape(2, 4, 2, 128), (3, 0, 2, 1)).reshape(128, 16)
    for i, l in enumerate(layers):
        pp[:, i, 0:8] = inp["norm_pre_mix"][l].reshape(8, 128).T
        pp[:, i, 8:16] = inp["norm_pre_ffn"][l].reshape(8, 128).T
        pp[:, i, 16:148] = np.transpose(inp["conv_w"][l].reshape(3, 44, 128), (2, 0, 1)).reshape(128, 132)
        pp[:, i, 148:192] = inp["conv_b"][l].reshape(44, 128).T
        pp[:, i, 192:208] = lbl
        pp[:, i, 208] = np.tile(inp["hgrn_norm"][l], 2)
        pp[:, i, 209] = inp["diff_norm"][l]
        lam_init = 0.8 - 0.6 * math.exp(-0.3 * l)
        pp[:, i, 210] = lam_init
        pp[:, i, 211] = 1.0 - lam_init
        pp[:, i, 212:468] = np.broadcast_to(inp["diff_lambda"][l].reshape(1, 256), (128, 256))
        pbc[i, 0] = np.broadcast_to(inp["norm_post_mix"][l][None, :], (128, D))
        pbc[i, 1] = np.broadcast_to(inp["norm_post_ffn"][l][None, :], (128, D))
    return pp, pbc


_NC_CACHE = {}


def kernel(x, w_in, w_out, lb_logits, hgrn_norm, diff_lambda, diff_norm, w_up, conv_w, conv_b,
           w_down, norm_pre_mix, norm_post_mix, norm_pre_ffn, norm_post_ffn):
    inp = dict(x=x, w_in=w_in, w_out=w_out, lb_logits=lb_logits, hgrn_norm=hgrn_norm, diff_lambda=diff_lambda,
               diff_norm=diff_norm, w_up=w_up, conv_w=conv_w, conv_b=conv_b, w_down=w_down, norm_pre_mix=norm_pre_mix,
               norm_post_mix=norm_post_mix, norm_pre_ffn=norm_pre_ffn, norm_post_ffn=norm_post_ffn)
    inp = {k: np.ascontiguousarray(np.asarray(v, dtype=np.float32)) for k, v in inp.items()}
    NL = 4
    if NL not in _NC_CACHE:
        _NC_CACHE[NL] = build(NL)
    nc = _NC_CACHE[NL]
    cosT, sinT, cm, cst = _consts()
    pp, pbc = _pack_params(inp, list(range(NL)))
    in_maps = []
    for c in range(8):
        b = c // 2
        in_maps.append({"x": inp["x"][b], "w_in": inp["w_in"], "w_out": inp["w_out"], "w_up": inp["w_up"], "w_down": inp["w_down"],
                        "pp": pp, "pbc": pbc, "cosT": cosT, "sinT": sinT, "cm": cm, "cst": cst})
    res = run_bass_kernel_spmd(nc, in_maps, core_ids=list(range(8)))
    out = np.stack([np.asarray(res.results[2 * b]["y"], dtype=np.float32) for b in range(4)], axis=0)
    return out
```
